# Optimizing a Trainium2 kernel written in Bass

```python
import jax
import jax.numpy as jnp
from jax import lax
import numpy as np

D_MODEL = 2048
BATCH = 4
SEQ = 2048
DEPTH = 2

MIX_WIDTH = D_MODEL
RWKV_WIDTH = MIX_WIDTH // 2
RWKV_HEAD_DIM = 64
RWKV_HEADS = RWKV_WIDTH // RWKV_HEAD_DIM
DECAY_LORA = 64
A_LORA = 64
GATE_LORA = 160
VRES_LORA = 32
MOBA_WIDTH = MIX_WIDTH - RWKV_WIDTH
MOBA_HEAD_DIM = 128
MOBA_HEADS = MOBA_WIDTH // MOBA_HEAD_DIM
MOBA_BLOCK = 256
MOBA_TOPK = 3
MOBA_Q_CHUNK = 16
D_FF = 5632
N_EXPERTS = 8
TOP_K = 2
D_FF_EXPERT = 5632
RMS_EPS = 1e-6
GN_EPS = 64e-5

kernel_name = "hybrid_rwkv7_moba_moe_adaln"


def rwkv_cols(value_residual):
    return 3 * RWKV_WIDTH + DECAY_LORA + A_LORA + GATE_LORA + (VRES_LORA if value_residual else 0)


def rms_norm(x, gain):
    xf = x.astype(jnp.float32)
    y = xf * lax.rsqrt(jnp.mean(xf * xf, axis=-1, keepdims=True) + RMS_EPS)
    return (y * gain.astype(jnp.float32)).astype(x.dtype)


def modulate(h, shift, scale):
    return h * (1 + scale[:, None, :]) + shift[:, None, :]


def token_shift(p, mu):
    prev = jnp.concatenate([jnp.zeros_like(p[:, :1]), p[:, :-1]], axis=1)
    return p + (prev - p) * mu


def rwkv7_recurrence(r, decay, k, v, a, b):
    B, T, H, N = r.shape

    def step(S, inp):
        r_t, w_t, k_t, v_t, a_t, b_t = inp
        sa = jnp.einsum('bhij,bhj->bhi', S, a_t)
        S = S * w_t[:, :, None, :] + sa[..., None] * b_t[:, :, None, :] + v_t[..., None] * k_t[:, :, None, :]
        return S, jnp.einsum('bhij,bhj->bhi', S, r_t)

    S0 = jnp.zeros((B, H, N, N), jnp.float32)
    xs = tuple(jnp.moveaxis(t.astype(jnp.float32), 1, 0) for t in (r, decay, k, v, a, b))
    _, ys = lax.scan(step, S0, xs)
    return jnp.moveaxis(ys, 0, 1)


def rwkv7_mixer(p, v_first, shift_mu, w0, w2, a0, a2, g2, v0, v2, k_k, k_a, r_k, ln_w, ln_b):
    B, T, _ = p.shape
    C, H, N = RWKV_WIDTH, RWKV_HEADS, RWKV_HEAD_DIM
    z = token_shift(p, shift_mu)
    sizes = [C, C, C, DECAY_LORA, A_LORA, GATE_LORA] + ([VRES_LORA] if v0 is not None else [])
    parts = jnp.split(z, np.cumsum(sizes)[:-1].tolist(), axis=-1)
    r, k, v, zw, za, zg = parts[:6]
    w = -jax.nn.softplus(-(w0 + jnp.tanh(zw) @ w2)) - 0.5
    decay = jnp.exp(-jnp.exp(w.astype(jnp.float32)))
    a = jax.nn.sigmoid(a0 + za @ a2)
    g = jax.nn.sigmoid(zg) @ g2
    if v0 is None:
        v_first = v
    else:
        v = v + (v_first - v) * jax.nn.sigmoid(v0 + parts[6] @ v2)
    heads = lambda t: t.reshape(B, T, H, N)
    kk = heads(k * k_k).astype(jnp.float32)
    kk = kk * lax.rsqrt(jnp.maximum(jnp.sum(kk * kk, axis=-1, keepdims=True), 1e-24))
    k = k * (1 + (a - 1) * k_a)
    a_h = heads(a).astype(jnp.float32)
    y = rwkv7_recurrence(heads(r), heads(decay), heads(k), heads(v), -kk, kk * a_h)
    mean = jnp.mean(y, axis=-1, keepdims=True)
    var = jnp.mean(jnp.square(y - mean), axis=-1, keepdims=True)
    y = ((y - mean) * lax.rsqrt(var + GN_EPS)).reshape(B, T, C) * ln_w + ln_b
    bonus = jnp.sum(heads(r) * heads(k) * r_k, axis=-1, keepdims=True) * heads(v)
    out = (y + bonus.reshape(B, T, C).astype(jnp.float32)) * g
    return out.astype(p.dtype), v_first


def moba_attention(q, k, v, gain):
    B, T, _ = q.shape
    H, Dh, BS, QC = MOBA_HEADS, MOBA_HEAD_DIM, MOBA_BLOCK, MOBA_Q_CHUNK
    to_heads = lambda t: t.reshape(B, T, H, Dh).transpose(0, 2, 1, 3)
    q, k, v = to_heads(q), to_heads(k), to_heads(v)
    n_blocks = -(-T // BS)
    pad = n_blocks * BS - T
    kp = jnp.pad(k, ((0, 0), (0, 0), (0, pad), (0, 0)))
    vp = jnp.pad(v, ((0, 0), (0, 0), (0, pad), (0, 0)))
    k_blocks = kp.reshape(B, H, n_blocks, BS, Dh)
    v_blocks = vp.reshape(B, H, n_blocks, BS, Dh)
    k_mean = jnp.mean(k_blocks.astype(jnp.float32), axis=3)
    topk = min(MOBA_TOPK, n_blocks)
    slopes = jnp.exp2(-8.0 * jnp.arange(1, H + 1, dtype=jnp.float32) / H)
    scale = Dh ** -0.5
    b_idx = jnp.arange(B)[:, None, None, None]
    h_idx = jnp.arange(H)[None, :, None, None]
    block_ids = jnp.arange(n_blocks)
    key_offsets = jnp.arange(BS)

    def chunk(ci):
        start = ci * QC
        own = start // BS
        qc = lax.dynamic_slice_in_dim(q, start, QC, axis=2)
        t_pos = (start + jnp.arange(QC))[:, None, None]
        gate = jnp.einsum('bhqd,bhnd->bhqn', qc.astype(jnp.float32), k_mean)
        gate = jnp.where(block_ids < own, gate, -jnp.inf)
        _, sel = lax.top_k(gate, topk)
        valid = (jnp.arange(topk) < own)[:, None]
        k_sel = k_blocks[b_idx, h_idx, sel]
        v_sel = v_blocks[b_idx, h_idx, sel]
        dist_sel = (t_pos - (sel[..., None] * BS + key_offsets)).astype(jnp.float32)
        s_sel = (jnp.einsum('bhqd,bhqksd->bhqks', qc, k_sel).astype(jnp.float32) * scale
                 - slopes[:, None, None, None] * dist_sel)
        s_sel = jnp.where(valid, s_sel, -jnp.inf).reshape(B, H, QC, topk * BS)
        k_own = lax.dynamic_slice_in_dim(kp, own * BS, BS, axis=2)
        v_own = lax.dynamic_slice_in_dim(vp, own * BS, BS, axis=2)
        dist_own = (t_pos[..., 0] - (own * BS + key_offsets)).astype(jnp.float32)
        s_own = (jnp.einsum('bhqd,bhsd->bhqs', qc, k_own).astype(jnp.float32) * scale
                 - slopes[:, None, None] * dist_own)
        s_own = jnp.where(dist_own >= 0, s_own, -jnp.inf)
        probs = jax.nn.softmax(jnp.concatenate([s_sel, s_own], axis=-1), axis=-1).astype(v.dtype)
        p_sel = probs[..., :topk * BS].reshape(B, H, QC, topk, BS)
        p_own = probs[..., topk * BS:]
        return (jnp.einsum('bhqks,bhqksd->bhqd', p_sel, v_sel)
                + jnp.einsum('bhqs,bhsd->bhqd', p_own, v_own))

    out = lax.map(chunk, jnp.arange(T // QC))
    out = out.transpose(1, 0, 3, 2, 4).reshape(B, T, H, Dh)
    out = rms_norm(out, gain.reshape(H, Dh))
    return out.reshape(B, T, MOBA_WIDTH)


def mixer_sublayer(h, v_first, p):
    n_r = p["shift_mu"].shape[0]
    proj = h @ p["w_in"]
    y_r, v_first = rwkv7_mixer(proj[..., :n_r], v_first, p["shift_mu"], p["w0"], p["w2"], p["a0"],
                               p["a2"], p["g2"], p.get("v0"), p.get("v2"), p["k_k"], p["k_a"],
                               p["r_k"], p["ln_w"], p["ln_b"])
    pm = proj[..., n_r:]
    y_m = moba_attention(pm[..., :MOBA_WIDTH], pm[..., MOBA_WIDTH:2 * MOBA_WIDTH],
                         pm[..., 2 * MOBA_WIDTH:], p["moba_gain"])
    return jnp.concatenate([y_r, y_m], axis=-1) @ p["w_out"], v_first


def swiglu(h, w_gate, w_up, w_down):
    return (jax.nn.silu(h @ w_gate) * (h @ w_up)) @ w_down


def moe_swiglu(h, router, w_gate, w_up, w_down):
    logits = (h @ router).astype(jnp.float32)
    top_val, top_idx = lax.top_k(logits, TOP_K)
    top_w = jax.nn.softmax(top_val, axis=-1)
    gates = jnp.sum(jax.nn.one_hot(top_idx, N_EXPERTS, dtype=jnp.float32) * top_w[..., None], axis=-2)
    out = jnp.zeros_like(h)
    for e in range(N_EXPERTS):
        out = out + gates[..., e:e + 1].astype(h.dtype) * swiglu(h, w_gate[e], w_up[e], w_down[e])
    return out


def setup_inputs(seed: int = 0) -> dict:
    key = jax.random.key(seed)
    keys = iter(jax.random.split(key, 96))
    normal = lambda shape, s: s * jax.random.normal(next(keys), shape, jnp.float32)
    uniform = lambda shape, lo, hi: jax.random.uniform(next(keys), shape, jnp.float32, lo, hi)
    D, C, H, N = D_MODEL, RWKV_WIDTH, RWKV_HEADS, RWKV_HEAD_DIM
    inp = {}
    inp["x"] = normal((BATCH, SEQ, D), 1.0)
    inp["c"] = normal((BATCH, D), 1.0)
    for i in range(DEPTH):
        pre = "l%d_" % i
        vres = i > 0
        inp[pre + "mod_w"] = normal((D, 6 * D), 0.5 * D ** -0.5)
        inp[pre + "mod_b"] = normal((6 * D,), 0.02)
        inp[pre + "norm_mix"] = 1.0 + normal((D,), 0.02)
        inp[pre + "w_in"] = normal((D, rwkv_cols(vres) + 3 * MOBA_WIDTH), D ** -0.5)
        inp[pre + "shift_mu"] = uniform((rwkv_cols(vres),), 0.0, 1.0)
        inp[pre + "w0"] = uniform((C,), -6.5, -1.5)
        inp[pre + "w2"] = normal((DECAY_LORA, C), 0.5 * DECAY_LORA ** -0.5)
        inp[pre + "a0"] = normal((C,), 0.1)
        inp[pre + "a2"] = normal((A_LORA, C), 0.5 * A_LORA ** -0.5)
        inp[pre + "g2"] = normal((GATE_LORA, C), GATE_LORA ** -0.5)
        if vres:
            inp[pre + "v0"] = normal((C,), 0.1)
            inp[pre + "v2"] = normal((VRES_LORA, C), 0.5 * VRES_LORA ** -0.5)
        inp[pre + "k_k"] = 0.85 + normal((C,), 0.02)
        inp[pre + "k_a"] = 1.0 + normal((C,), 0.02)
        inp[pre + "r_k"] = normal((H, N), 0.1)
        inp[pre + "ln_w"] = 1.0 + normal((C,), 0.02)
        inp[pre + "ln_b"] = normal((C,), 0.02)
        inp[pre + "moba_gain"] = 1.0 + normal((MOBA_WIDTH,), 0.02)
        inp[pre + "w_out"] = normal((MIX_WIDTH, D), MIX_WIDTH ** -0.5)
        inp[pre + "norm_ffn"] = 1.0 + normal((D,), 0.02)
        if i % 2 == 0:
            inp[pre + "ffn_gate"] = normal((D, D_FF), D ** -0.5)
            inp[pre + "ffn_up"] = normal((D, D_FF), D ** -0.5)
            inp[pre + "ffn_down"] = normal((D_FF, D), D_FF ** -0.5)
        else:
            inp[pre + "router"] = normal((D, N_EXPERTS), D ** -0.5)
            inp[pre + "exp_gate"] = normal((N_EXPERTS, D, D_FF_EXPERT), D ** -0.5)
            inp[pre + "exp_up"] = normal((N_EXPERTS, D, D_FF_EXPERT), D ** -0.5)
            inp[pre + "exp_down"] = normal((N_EXPERTS, D_FF_EXPERT, D), D_FF_EXPERT ** -0.5)
    inp["norm_out"] = 1.0 + normal((D,), 0.02)
    return inp


def reference(x, c,
              l0_mod_w, l0_mod_b, l0_norm_mix, l0_w_in, l0_shift_mu, l0_w0, l0_w2, l0_a0, l0_a2, l0_g2,
              l0_k_k, l0_k_a, l0_r_k, l0_ln_w, l0_ln_b, l0_moba_gain, l0_w_out, l0_norm_ffn,
              l0_ffn_gate, l0_ffn_up, l0_ffn_down,
              l1_mod_w, l1_mod_b, l1_norm_mix, l1_w_in, l1_shift_mu, l1_w0, l1_w2, l1_a0, l1_a2, l1_g2,
              l1_v0, l1_v2, l1_k_k, l1_k_a, l1_r_k, l1_ln_w, l1_ln_b, l1_moba_gain, l1_w_out, l1_norm_ffn,
              l1_router, l1_exp_gate, l1_exp_up, l1_exp_down,
              norm_out):
    layers = (
        dict(mod_w=l0_mod_w, mod_b=l0_mod_b, norm_mix=l0_norm_mix, w_in=l0_w_in, shift_mu=l0_shift_mu,
             w0=l0_w0, w2=l0_w2, a0=l0_a0, a2=l0_a2, g2=l0_g2, k_k=l0_k_k, k_a=l0_k_a, r_k=l0_r_k,
             ln_w=l0_ln_w, ln_b=l0_ln_b, moba_gain=l0_moba_gain, w_out=l0_w_out, norm_ffn=l0_norm_ffn,
             ffn_gate=l0_ffn_gate, ffn_up=l0_ffn_up, ffn_down=l0_ffn_down),
        dict(mod_w=l1_mod_w, mod_b=l1_mod_b, norm_mix=l1_norm_mix, w_in=l1_w_in, shift_mu=l1_shift_mu,
             w0=l1_w0, w2=l1_w2, a0=l1_a0, a2=l1_a2, g2=l1_g2, v0=l1_v0, v2=l1_v2, k_k=l1_k_k,
             k_a=l1_k_a, r_k=l1_r_k, ln_w=l1_ln_w, ln_b=l1_ln_b, moba_gain=l1_moba_gain, w_out=l1_w_out,
             norm_ffn=l1_norm_ffn, router=l1_router, exp_gate=l1_exp_gate, exp_up=l1_exp_up,
             exp_down=l1_exp_down),
    )
    c_act = jax.nn.silu(c)
    v_first = None
    for i in range(DEPTH):
        p = layers[i]
        mod = c_act @ p["mod_w"] + p["mod_b"]
        shift_m, scale_m, gate_m, shift_f, scale_f, gate_f = jnp.split(mod, 6, axis=-1)
        h = modulate(rms_norm(x, p["norm_mix"]), shift_m, scale_m)
        mix, v_first = mixer_sublayer(h, v_first, p)
        x = x + gate_m[:, None, :] * mix
        h = modulate(rms_norm(x, p["norm_ffn"]), shift_f, scale_f)
        if i % 2 == 0:
            f = swiglu(h, p["ffn_gate"], p["ffn_up"], p["ffn_down"])
        else:
            f = moe_swiglu(h, p["router"], p["exp_gate"], p["exp_up"], p["exp_down"])
        x = x + gate_f[:, None, :] * f
    return rms_norm(x, norm_out)
```

```python
import numpy as np
from contextlib import ExitStack
import concourse.bass as bass
import concourse.mybir as mybir
from concourse.bass_utils import run_bass_kernel_spmd

F32 = mybir.dt.float32
BF16 = mybir.dt.bfloat16
AF = mybir.ActivationFunctionType
ALU = mybir.AluOpType
AX = mybir.AxisListType

T = 2048
D = 2048
NT = T // 128
DC = D // 128
C = 1024
DFF = 5632
FC = DFF // 128
NEXP = 8
NB = 8
SCALE = 128 ** -0.5
BIGR = 1.0e5
N_CORES = 4


class Buf:
    __slots__ = ("w", "r")

    def __init__(self):
        self.w = None
        self.r = {}


class Tile:
    def __init__(self, t, shape):
        self.t = t
        self.b = Buf()
        self.shape = shape

    def __getitem__(self, k):
        return self.t[k]


class Sched:
    LIMIT = 60000
    NDS = 24

    def __init__(self, nc, es):
        self.nc = nc
        self.es = es
        self.E = {"pe": nc.tensor, "act": nc.scalar, "dve": nc.vector, "pool": nc.gpsimd, "sp": nc.sync}
        self.semobj = {}
        self.cur = {}
        self.cnt = {}
        self.nsem = 0
        self.known = {e: {} for e in self.E}
        self.latest = {}
        for e in ("pe", "act", "dve", "pool"):
            self._newsem(e)
        self.dsem = {q: [self._alloc("d%s%d" % (q, i)) for i in range(self.NDS)] for q in ("sp", "pool")}
        self.duse = {q: [0] * self.NDS for q in ("sp", "pool")}
        self.dnext = {"sp": 0, "pool": 0}
        self.n_ins = 0

    def _alloc(self, name):
        s = self.es.enter_context(self.nc.semaphore(name))
        k = self.nsem
        self.nsem += 1
        self.semobj[k] = s
        return k

    def _newsem(self, e):
        k = self._alloc("%s_%d" % (e, self.nsem))
        self.cur[e] = k
        self.cnt[e] = 0

    def _wait(self, e, deps):
        kn = self.known[e]
        best = {}
        for (k, v) in deps:
            if best.get(k, 0) < v:
                best[k] = v
        for k, v in best.items():
            if e == "pe" and k == self.cur["pe"]:
                continue
            if kn.get(k, 0) >= v:
                continue
            self.E[e].wait_ge(self.semobj[k], v)
            kn[k] = v
            self.n_ins += 1

    def _deps(self, reads, writes):
        deps = []
        for b in reads:
            if b.w is not None:
                deps.append(b.w)
        for b in writes:
            if b.w is not None:
                deps.append(b.w)
            deps.extend(b.r.items())
        return deps

    def _mark(self, ev, reads, writes):
        for b in reads:
            if b.r.get(ev[0], 0) < ev[1]:
                b.r[ev[0]] = ev[1]
        for b in writes:
            b.w = ev
            b.r = {}
        self.latest[ev[0]] = ev[1]

    def op(self, e, fn, reads=(), writes=()):
        reads = [x.b if isinstance(x, Tile) else x for x in reads]
        writes = [x.b if isinstance(x, Tile) else x for x in writes]
        self._wait(e, self._deps(reads, writes))
        if self.cnt[e] >= self.LIMIT:
            self._newsem(e)
        ins = fn(self.E[e])
        k = self.cur[e]
        ins.then_inc(self.semobj[k], 1)
        self.cnt[e] += 1
        self.n_ins += 1
        self._mark((k, self.cnt[e]), reads, writes)

    def dma(self, q, out, in_, reads=(), writes=(), **kw):
        reads = [x.b if isinstance(x, Tile) else x for x in reads]
        writes = [x.b if isinstance(x, Tile) else x for x in writes]
        i = self.dnext[q]
        self.dnext[q] = (i + 1) % self.NDS
        dsem, duse = self.dsem[q], self.duse[q]
        deps = self._deps(reads, writes)
        if duse[i] > 0:
            deps.append((dsem[i], 16 * duse[i]))
        self._wait(q, deps)
        ins = self.E[q].dma_start(out=out, in_=in_, **kw)
        ins.then_inc(self.semobj[dsem[i]], 16)
        duse[i] += 1
        self.n_ins += 1
        self._mark((dsem[i], 16 * duse[i]), reads, writes)

    def barrier(self, engines=("pe", "act", "dve", "pool", "sp")):
        evs = list(self.latest.items())
        for e in engines:
            self._wait(e, evs)


class Ctx:
    pass


class StopBuild(Exception):
    pass


def build_program(n_layers=2, dbg=(), stop=None):
    nc = bass.Bass("TRN2", target_bir_lowering=False)
    es = ExitStack()
    S = Sched(nc, es)
    G = Ctx()
    G.nc, G.S, G.es = nc, S, es

    def din(name, shape, dt=F32):
        return nc.dram_tensor(name, list(shape), dt, kind="ExternalInput").ap()

    def dscr(name, shape, dt=F32):
        if name in dbg:
            return nc.dram_tensor(name, list(shape), dt, kind="ExternalOutput").ap()
        return nc.dram_tensor(name, list(shape), dt).ap()
    G.stop = stop

    G.x_in = din("x", [T, D])
    G.consts = din("consts", [128, NCONST])
    G.rowp = din("rowp", [128, NROW])
    G.out = nc.dram_tensor("out", [T, D], F32, kind="ExternalOutput").ap()
    G.W = []
    for l in range(n_layers):
        vr = l > 0
        ncols = 3360 + (32 if vr else 0) + 3072
        w = {}
        w["vecs"] = din("l%d_vecs" % l, [128, NVEC])
        w["mod_w"] = din("l%d_mod_w" % l, [D, 6 * D])
        w["w_in"] = din("l%d_w_in" % l, [D, ncols])
        w["w2"] = din("l%d_w2" % l, [64, C])
        w["a2"] = din("l%d_a2" % l, [64, C])
        w["g2"] = din("l%d_g2" % l, [160, C])
        if vr:
            w["v2"] = din("l%d_v2" % l, [32, C])
        w["w_out"] = din("l%d_w_out" % l, [D, D])
        if l % 2 == 0:
            w["ffn_gate"] = din("l%d_ffn_gate" % l, [1, D, DFF])
            w["ffn_up"] = din("l%d_ffn_up" % l, [1, D, DFF])
            w["ffn_down"] = din("l%d_ffn_down" % l, [1, DFF, D])
        else:
            import os
            nin = int(os.environ.get("DEV_NEXP", NEXP))
            w["router"] = din("l%d_router" % l, [128, 16 * NEXP])
            w["ffn_gate"] = din("l%d_exp_gate" % l, [nin, D, DFF])
            w["ffn_up"] = din("l%d_exp_up" % l, [nin, D, DFF])
            w["ffn_down"] = din("l%d_exp_down" % l, [nin, DFF, D])
        w["ncols"] = ncols
        w["vr"] = vr
        G.W.append(w)

    G.xa = dscr("xa", [T, D])
    G.xb = dscr("xb", [T, D])
    G.hT = dscr("hT", [D, T], BF16)
    G.zf = dscr("zf", [6464, T])
    G.rw = {k: dscr("rw_" + k, [C, T]) for k in ("w", "kmod", "v", "a", "b", "g", "bonus", "y")}
    G.vtm = dscr("vtm", [T, C], BF16)
    G.vfirst = dscr("vfirst", [C, T])
    G.mixT = dscr("mixT", [D, T], BF16)
    G.gT = dscr("gT", [NEXP, T])
    G.dbg = {}
    if "mod" in dbg:
        G.dbg["mod"] = nc.dram_tensor("dbg_mod", [128, 96], F32, kind="ExternalOutput").ap()

    G.uid = 0

    def sb(name, shape, dt=F32, stack=None):
        G.uid += 1
        name = "%s_u%d" % (name, G.uid)
        t = (stack or es).enter_context(nc.sbuf_tensor(name, list(shape), dt))
        return Tile(t, shape)

    def ps(name, shape, dt=F32, stack=None):
        G.uid += 1
        name = "%s_u%d" % (name, G.uid)
        full = (stack or es).enter_context(nc.psum_tensor(name, [128, 512], F32))
        shape = list(shape)
        n = 1
        for d_ in shape[1:]:
            n *= d_
        assert n <= 512 and dt == F32
        v = full[0:shape[0], 0:n]
        if len(shape) == 3:
            v = v.rearrange("p (a b) -> p a b", a=shape[1])
        elif len(shape) == 4:
            v = v.rearrange("p (a b c) -> p a b c", a=shape[1], b=shape[2])
        return Tile(v, shape)

    G.sb, G.ps = sb, ps
    G.cst = sb("cst", [128, NCONST])
    S.dma("sp", G.cst[:, :], G.consts, writes=[G.cst])
    G.ident = G.cst.t[:, CO_IDENT:CO_IDENT + 128]
    G.bones = G.cst.t[:, CO_BONES:CO_BONES + 128]
    G.cstb = sb("cstb", [128, NCONST], BF16)
    S.op("act", lambda e: e.copy(out=G.cstb[:, :], in_=G.cst[:, :]), reads=[G.cst], writes=[G.cstb])
    G.identb = G.cstb.t[:, CO_IDENT:CO_IDENT + 128]

    try:
        for l in range(n_layers):
            build_layer(G, l)
        phase_final(G, G.xb)
    except StopBuild:
        pass
    S.barrier()
    G.n_ins = S.n_ins
    build_program.last = G
    return nc


CO_IDENT = 0
CO_BONES = 128
CO_ONE32 = 256
CO_SELC = 289
CO_TROW = 305
CO_CM0 = CO_TROW + T
CO_CM1 = CO_CM0 + 256
CO_COLB = CO_CM1 + 256
CO_PAST = CO_COLB + 128
CO_M0 = CO_PAST + 64
CO_EPS6 = CO_M0 + 2
CO_EPSGN = CO_M0 + 3
NCONST = CO_M0 + 4


def make_consts():
    c = np.zeros((128, NCONST), np.float32)
    c[:, CO_IDENT:CO_IDENT + 128] = np.eye(128, dtype=np.float32)
    bo = np.zeros((128, 128), np.float32)
    bo[:64, :64] = 1
    bo[64:, 64:] = 1
    c[:, CO_BONES:CO_BONES + 128] = bo
    c[:, CO_ONE32 + 32] = 1.0
    for n in range(8):
        c[n, CO_SELC + n] = 1.0
        c[32, CO_SELC + n] = 1.0
    c[32, CO_SELC + 8] = 1.0
    c[32, CO_TROW:CO_TROW + T] = np.arange(T, dtype=np.float32)
    s = np.arange(128)[:, None]
    t = np.arange(256)[None, :]
    c[:, CO_CM0:CO_CM0 + 256] = np.where(t >= s, 0.0, -BIGR)
    c[:, CO_CM1:CO_CM1 + 256] = np.where(t >= s + 128, 0.0, -BIGR)
    slopes = np.exp2(-8.0 * np.arange(1, 9, dtype=np.float32) / 8).astype(np.float32)
    for h in range(8):
        for st in range(16):
            c[:, CO_COLB + h * 16 + st] = slopes[h] * (st * 128 + np.arange(128, dtype=np.float32))
    for own in range(8):
        for n in range(8):
            c[:, CO_PAST + own * 8 + n] = 0.0 if n < own else -1.0e30
    c[:64, CO_M0] = 1.0
    c[64:, CO_M0 + 1] = 1.0
    c[:, CO_EPS6] = 1e-6
    c[:, CO_EPSGN] = 64e-5
    return c


SLOPES = [float(2.0 ** (-8.0 * (h + 1) / 8)) for h in range(8)]

VO = {}
_o = 0
for _n, _k in (("c", 16), ("mod_b", 96), ("norm_mix", 16), ("norm_ffn", 16), ("mu_r", 8), ("mu_k", 8), ("mu_v", 8),
               ("mu_zw", 1), ("mu_za", 1), ("mu_zg", 2), ("mu_zv", 1), ("mu_zg2", 1), ("w0", 8), ("a0", 8), ("v0", 8), ("k_k", 8),
               ("k_a", 8), ("r_k", 8), ("ln_w", 8), ("ln_b", 8)):
    VO[_n] = _o
    _o += _k
NVEC = _o

RO_GAIN = [0, 1024]
RO_NOUT = 2048
NROW = 4096


def cols(v, k):
    v = np.asarray(v, np.float32).reshape(-1)
    o = np.zeros((128, k), np.float32)
    n = v.shape[0]
    full = n // 128
    if full:
        o[:, :full] = v[:full * 128].reshape(full, 128).T
    rem = n - full * 128
    if rem:
        o[:rem, full] = v[full * 128:]
    return o


def make_vecs(inp, l, b):
    p = "l%d_" % l
    v = np.zeros((128, NVEC), np.float32)

    def put(name, arr, k):
        v[:, VO[name]:VO[name] + k] = cols(arr, k)

    put("c", inp["c"][b], 16)
    put("mod_b", inp[p + "mod_b"], 96)
    put("norm_mix", inp[p + "norm_mix"], 16)
    put("norm_ffn", inp[p + "norm_ffn"], 16)
    mu = inp[p + "shift_mu"]
    put("mu_r", mu[0:1024], 8)
    put("mu_k", mu[1024:2048], 8)
    put("mu_v", mu[2048:3072], 8)
    put("mu_zw", mu[3072:3136], 1)
    put("mu_za", mu[3136:3200], 1)
    put("mu_zg", mu[3200:3328], 1)
    put("mu_zg2", mu[3328:3360], 1)
    if l > 0:
        put("mu_zv", mu[3360:3392], 1)
        put("v0", inp[p + "v0"], 8)
    put("w0", inp[p + "w0"], 8)
    put("a0", inp[p + "a0"], 8)
    put("k_k", inp[p + "k_k"], 8)
    put("k_a", inp[p + "k_a"], 8)
    put("r_k", inp[p + "r_k"].reshape(-1), 8)
    put("ln_w", inp[p + "ln_w"], 8)
    put("ln_b", inp[p + "ln_b"], 8)
    return v


def make_rowp(inp):
    r = np.zeros((128, NROW), np.float32)
    r[:, 0:1024] = np.broadcast_to(inp["l0_moba_gain"][None, :], (128, 1024))
    r[:, 1024:2048] = np.broadcast_to(inp["l1_moba_gain"][None, :], (128, 1024))
    r[:, 2048:4096] = np.broadcast_to(inp["norm_out"][None, :], (128, 2048))
    return r


def build_layer(G, l):
    S = G.S
    W = G.W[l]
    x_in = G.x_in if l == 0 else G.xb
    L = Ctx()
    L.l = l
    L.W = W
    S.barrier()
    with ExitStack() as ls:
        L.vec = G.sb("vec%d" % l, [128, NVEC], stack=ls)
        S.dma("sp", L.vec[:, :], W["vecs"], writes=[L.vec])
        L.modv = G.sb("modv%d" % l, [128, 96], stack=ls)
        L.der = G.sb("der%d" % l, [128, 96], stack=ls)
        L.om = G.sb("om%d" % l, [128, NVEC], stack=ls)
        seq = [("mod", lambda: phase_mod(G, L)),
               ("norm1", lambda: phase_norm(G, L, x_in, 0, 16, router=None)),
               ("proj", lambda: phase_proj(G, L)),
               ("pre", lambda: phase_rwkv_pre(G, L)),
               ("rec", lambda: phase_rec(G, L)),
               ("post", lambda: phase_rwkv_post(G, L)),
               ("moba", lambda: phase_moba(G, L)),
               ("wout", lambda: phase_wout(G, L, x_in)),
               ("norm2", lambda: phase_norm(G, L, G.xa, 48, 64, router=(W.get("router")))),
               ("ffn", lambda: phase_ffn(G, L))]
        skip = getattr(G, "skip", ())
        for name, fn in seq:
            if name not in skip and (l, name) not in skip:
                fn()
                S.barrier()
            if G.stop == (l, name):
                raise StopBuild()
        S.barrier()


def V(L, name, j=0, n=1, p0=0, p1=128):
    o = VO[name] + j
    return L.vec.t[p0:p1, o:o + n]


def phase_mod(G, L):
    S, nc, W = G.S, G.nc, L.W
    with ExitStack() as st:
        cs = G.sb("cs", [128, 16], BF16, stack=st)
        S.op("act", lambda e: e.activation(out=cs[:, :], in_=V(L, "c", 0, 16), func=AF.Silu), reads=[L.vec], writes=[cs])
        wt = [G.sb("modwt%d" % i, [128, 16, 512], BF16, stack=st) for i in range(2)]
        pm = G.ps("pmod", [128, 96], stack=st)
        mw = W["mod_w"].rearrange("(k p) n -> p k n", p=128)
        for j4 in range(24):
            w = wt[j4 % 2]
            S.dma("pool", w[:, :, :], mw[:, :, j4 * 512:(j4 + 1) * 512], writes=[w])
            for jj in range(4):
                j = j4 * 4 + jj
                for k in range(16):
                    S.op("pe", lambda e, w=w, jj=jj, k=k, j=j: e.matmul(pm[:, j:j + 1], w[:, k, jj * 128:(jj + 1) * 128], cs[:, k:k + 1],
                                                                      start=(k == 0), stop=(k == 15)),
                         reads=[w, cs], writes=[pm])
        S.op("dve", lambda e: e.tensor_tensor(out=L.modv[:, :], in0=pm[:, :], in1=V(L, "mod_b", 0, 96), op=ALU.add),
             reads=[pm, L.vec], writes=[L.modv])
        mv, dr = L.modv, L.der
        S.op("dve", lambda e: e.scalar_tensor_tensor(out=dr[:, 0:16], in0=mv[:, 16:32], scalar=1.0, in1=V(L, "norm_mix", 0, 16),
                                                     op0=ALU.add, op1=ALU.mult), reads=[mv, L.vec], writes=[dr])
        S.op("dve", lambda e: e.tensor_copy(out=dr[:, 16:32], in_=mv[:, 0:16]), reads=[mv], writes=[dr])
        S.op("dve", lambda e: e.tensor_copy(out=dr[:, 32:48], in_=mv[:, 32:48]), reads=[mv], writes=[dr])
        S.op("dve", lambda e: e.scalar_tensor_tensor(out=dr[:, 48:64], in0=mv[:, 64:80], scalar=1.0, in1=V(L, "norm_ffn", 0, 16),
                                                     op0=ALU.add, op1=ALU.mult), reads=[mv, L.vec], writes=[dr])
        S.op("dve", lambda e: e.tensor_copy(out=dr[:, 64:80], in_=mv[:, 48:64]), reads=[mv], writes=[dr])
        S.op("dve", lambda e: e.tensor_copy(out=dr[:, 80:96], in_=mv[:, 80:96]), reads=[mv], writes=[dr])
        S.op("dve", lambda e: e.tensor_scalar(out=L.om[:, :], in0=L.vec[:, :], scalar1=-1.0, scalar2=1.0, op0=ALU.mult, op1=ALU.add),
             reads=[L.vec], writes=[L.om])
        if "mod" in G.dbg and L.l == 0:
            S.dma("sp", G.dbg["mod"], L.modv[:, :], reads=[L.modv])
        S.barrier()


def phase_norm(G, L, x_dram, ao, bo, router=None):
    S, nc = G.S, G.nc
    with ExitStack() as st:
        xt = [G.sb("nx%d" % i, [128, D], stack=st) for i in range(2)]
        sq = G.sb("nsq", [128, D], stack=st)
        xn = [G.sb("nxn%d" % i, [128, D], stack=st) for i in range(2)]
        ss = [G.sb("nss%d" % i, [128, 2], stack=st) for i in range(2)]
        hst = [G.sb("nhs%d" % i, [128, 16, 128], BF16, stack=st) for i in range(2)]
        pt = [G.ps("npt%d" % i, [128, 512], stack=st) for i in range(2)]
        hview = G.hT.rearrange("(k p) t -> p k t", p=128)
        import os
        RL = int(os.environ.get("ROUTER_LVL", 9))
        if router is not None:
            rsb = G.sb("rsb", [128, 16, NEXP], stack=st)
            S.dma("sp", rsb[:, :, :], router.rearrange("p (k e) -> p k e", k=16), writes=[rsb])
            hf = [G.sb("nhf%d" % i, [128, 16, 128], stack=st) for i in range(2)]
            plg = G.ps("nplg", [128, NEXP], stack=st)
            lg = G.sb("nlg", [128, NEXP], stack=st)
            t8 = G.sb("nt8", [128, 8], stack=st)
            sm = G.sb("nsm", [128, 8], stack=st)
            ge = G.sb("nge", [128, NEXP], stack=st)
            gsel = G.sb("ngsel", [128, NEXP], stack=st)
            pgt = G.ps("npgt", [NEXP, 128], stack=st)
            gts = G.sb("ngts", [NEXP, T], stack=st)
        for tt in range(NT):
            x = xt[tt % 2]
            n_ = xn[tt % 2]
            s_ = ss[tt % 2]
            S.dma("sp", x[:, :], x_dram[tt * 128:(tt + 1) * 128, :], writes=[x])
            S.op("act", lambda e, x=x: e.activation(out=sq[:, :], in_=x[:, :], func=AF.Square), reads=[x], writes=[sq])
            S.op("dve", lambda e, s_=s_: e.tensor_reduce(out=s_[:, 0:1], in_=sq[:, :], axis=AX.X, op=ALU.add), reads=[sq], writes=[s_])
            S.op("act", lambda e, s_=s_: e.activation(out=s_[:, 1:2], in_=s_[:, 0:1], func=AF.Sqrt, scale=1.0 / D, bias=G.cst.t[:, CO_EPS6:CO_EPS6 + 1]),
                 reads=[s_, G.cst], writes=[s_])
            S.op("dve", lambda e, s_=s_: e.reciprocal(out=s_[:, 0:1], in_=s_[:, 1:2]), reads=[s_], writes=[s_])
            S.op("act", lambda e, x=x, n_=n_, s_=s_: e.activation(out=n_[:, :], in_=x[:, :], func=AF.Copy, scale=s_[:, 0:1]),
                 reads=[x, s_], writes=[n_])
            hs = hst[tt % 2]
            for k4 in range(4):
                p_ = pt[k4 % 2]
                for kk in range(4):
                    k = k4 * 4 + kk
                    S.op("pe", lambda e, p_=p_, kk=kk, k=k, n_=n_: e.transpose(out=p_[:, kk * 128:(kk + 1) * 128], in_=n_[:, k * 128:(k + 1) * 128],
                                                                              identity=G.ident), reads=[n_, G.cst], writes=[p_])
                for kk in range(4):
                    k = k4 * 4 + kk
                    S.op("act", lambda e, p_=p_, kk=kk, k=k, hs=hs: e.activation(out=hs[:, k, :], in_=p_[:, kk * 128:(kk + 1) * 128], func=AF.Identity,
                                                                                scale=L.der[:, ao + k:ao + k + 1], bias=L.der[:, bo + k:bo + k + 1]),
                         reads=[p_, L.der], writes=[hs])
                    if router is not None and RL >= 2:
                        h_ = hf[tt % 2]
                        S.op("act", lambda e, p_=p_, kk=kk, k=k, h_=h_: e.activation(out=h_[:, k, :], in_=p_[:, kk * 128:(kk + 1) * 128], func=AF.Identity,
                                                                                    scale=L.der[:, ao + k:ao + k + 1], bias=L.der[:, bo + k:bo + k + 1]),
                             reads=[p_, L.der], writes=[h_])
            if router is not None and RL >= 2:
                h_ = hf[tt % 2]
                for k in range(16):
                    S.op("pe", lambda e, k=k, h_=h_: e.matmul(plg[:, :], h_[:, k, :], rsb[:, k, :], start=(k == 0), stop=(k == 15)),
                         reads=[h_, rsb], writes=[plg])
            S.dma("sp", hview[:, :, tt * 128:(tt + 1) * 128], hs[:, :, :], reads=[hs])
            if router is not None and RL >= 3:
                S.op("dve", lambda e: e.tensor_copy(out=lg[:, :], in_=plg[:, :]), reads=[plg], writes=[lg])
                S.op("dve", lambda e: e.max(out=t8[:, :], in_=lg[:, :]), reads=[lg], writes=[t8])
            if router is not None and RL >= 4:
                S.op("dve", lambda e: e.tensor_scalar(out=sm[:, 0:1], in0=t8[:, 0:1], scalar1=-1.0, scalar2=None, op0=ALU.mult), reads=[t8], writes=[sm])
                S.op("act", lambda e: e.activation(out=sm[:, 1:2], in_=t8[:, 1:2], func=AF.Exp, bias=sm[:, 0:1]), reads=[t8, sm], writes=[sm])
                S.op("dve", lambda e: e.tensor_scalar(out=sm[:, 3:4], in0=sm[:, 1:2], scalar1=1.0, scalar2=None, op0=ALU.add), reads=[sm], writes=[sm])
                S.op("dve", lambda e: e.reciprocal(out=sm[:, 2:3], in_=sm[:, 3:4]), reads=[sm], writes=[sm])
                S.op("act", lambda e: e.activation(out=ge[:, :], in_=lg[:, :], func=AF.Exp, bias=sm[:, 0:1]), reads=[lg, sm], writes=[ge])
                S.op("dve", lambda e: e.tensor_scalar(out=gsel[:, :], in0=lg[:, :], scalar1=t8[:, 1:2], scalar2=sm[:, 2:3], op0=ALU.is_ge, op1=ALU.mult),
                     reads=[lg, t8, sm], writes=[gsel])
                S.op("dve", lambda e: e.tensor_tensor(out=ge[:, :], in0=ge[:, :], in1=gsel[:, :], op=ALU.mult), reads=[ge, gsel], writes=[ge])
            if router is not None and RL >= 5:
                S.op("pe", lambda e: e.transpose(out=pgt[:, :], in_=ge[:, :], identity=G.ident), reads=[ge, G.cst], writes=[pgt])
                S.op("act", lambda e, tt=tt: e.copy(out=gts[:, tt * 128:(tt + 1) * 128], in_=pgt[:, :]), reads=[pgt], writes=[gts])
        if router is not None and RL >= 6:
            S.dma("sp", G.gT, gts[:, :], reads=[gts])
        S.barrier()


def proj_chunks(W):
    ch = []
    for i in range(8):
        ch.append((i * 128, 128, i * 128, "mu_r", i))
    for i in range(8):
        ch.append((1024 + i * 128, 128, 1024 + i * 128, "mu_k", i))
    for i in range(8):
        ch.append((2048 + i * 128, 128, 2048 + i * 128, "mu_v", i))
    ch.append((3072, 64, 3072, "mu_zw", 0))
    ch.append((3136, 64, 3136, "mu_za", 0))
    ch.append((3200, 128, 3200, "mu_zg", 0))
    ch.append((3328, 32, 3328, "mu_zg2", 0))
    nr = 3360
    if W["vr"]:
        ch.append((3360, 32, 3360, "mu_zv", 0))
        nr = 3392
    for i in range(24):
        ch.append((nr + i * 128, 128, 3392 + i * 128, None, 0))
    return ch


ZQ = 3392


def phase_proj(G, L):
    S, nc, W = G.S, G.nc, L.W
    with ExitStack() as st:
        hT = G.sb("pj_hT", [128, 16, T], BF16, stack=st)
        hb = [Buf() for _ in range(4)]
        hview = G.hT.rearrange("(k p) t -> p k t", p=128)
        for q in range(4):
            S.dma("sp", hT[:, :, q * 512:(q + 1) * 512], hview[:, :, q * 512:(q + 1) * 512], writes=[hb[q]])
        wt = [G.sb("pj_w%d" % i, [128, 16, 128], BF16, stack=st) for i in range(2)]
        stg = [G.sb("pj_s%d" % i, [128, T + 1], stack=st) for i in range(2)]
        tmp = G.sb("pj_tmp", [128, T], stack=st)
        zt = [G.sb("pj_z%d" % i, [128, T], stack=st) for i in range(2)]
        pp = [G.ps("pj_p%d" % i, [128, 512], stack=st) for i in range(2)]
        for s_ in stg:
            S.op("dve", lambda e, s_=s_: e.memset(s_[:, 0:1], 0.0), writes=[s_])
        wv = W["w_in"].rearrange("(k p) n -> p k n", p=128)
        for ci, (c0, n, r0, mu, mj) in enumerate(proj_chunks(W)):
            w = wt[ci % 2]
            sg = stg[ci % 2]
            S.dma("pool", w[:, :, 0:n], wv[:, :, c0:c0 + n], writes=[w])
            for q in range(4):
                p_ = pp[q % 2]
                for k in range(16):
                    S.op("pe", lambda e, p_=p_, w=w, k=k, q=q, n=n: e.matmul(p_[0:n, :], w[:, k, 0:n], hT[:, k, q * 512:(q + 1) * 512],
                                                                            start=(k == 0), stop=(k == 15)), reads=[w, hb[q]], writes=[p_])
                S.op("act", lambda e, p_=p_, sg=sg, q=q, n=n: e.copy(out=sg[0:n, 1 + q * 512:1 + (q + 1) * 512], in_=p_[0:n, :]), reads=[p_], writes=[sg])
            if mu is not None:
                z = zt[ci % 2]
                S.op("dve", lambda e, sg=sg, n=n, mu=mu, mj=mj: e.tensor_scalar(out=tmp[0:n, :], in0=sg[0:n, 1:T + 1], scalar1=L.om[0:n, VO[mu] + mj:VO[mu] + mj + 1],
                                                                               scalar2=None, op0=ALU.mult), reads=[sg, L.om], writes=[tmp])
                S.op("dve", lambda e, sg=sg, n=n, mu=mu, mj=mj, z=z: e.scalar_tensor_tensor(out=z[0:n, :], in0=sg[0:n, 0:T], scalar=V(L, mu, mj, 1, 0, n), in1=tmp[0:n, :],
                                                                                          op0=ALU.mult, op1=ALU.add), reads=[sg, L.vec, tmp], writes=[z])
                S.dma("sp", G.zf[r0:r0 + n, :], z[0:n, :], reads=[z])
            else:
                S.dma("sp", G.zf[r0:r0 + n, :], sg[0:n, 1:T + 1], reads=[sg])
        S.barrier()


def phase_rwkv_pre(G, L):
    S, nc, W = G.S, G.nc, L.W
    vr = W["vr"]
    with ExitStack() as st:
        tzw = G.sb("rp_tzw", [64, T], stack=st)
        za = G.sb("rp_za", [64, T], stack=st)
        sga = G.sb("rp_sga", [128, T], stack=st)
        sgb = G.sb("rp_sgb", [32, T], stack=st)
        S.dma("sp", tzw[:, :], G.zf[3072:3136, :], writes=[tzw])
        S.dma("sp", za[:, :], G.zf[3136:3200, :], writes=[za])
        S.dma("sp", sga[:, :], G.zf[3200:3328, :], writes=[sga])
        S.dma("sp", sgb[:, :], G.zf[3328:3360, :], writes=[sgb])
        S.op("act", lambda e: e.activation(out=tzw[:, :], in_=tzw[:, :], func=AF.Tanh), reads=[tzw], writes=[tzw])
        S.op("act", lambda e: e.activation(out=sga[:, :], in_=sga[:, :], func=AF.Sigmoid), reads=[sga], writes=[sga])
        S.op("act", lambda e: e.activation(out=sgb[:, :], in_=sgb[:, :], func=AF.Sigmoid), reads=[sgb], writes=[sgb])
        w2 = G.sb("rp_w2", [64, C], stack=st)
        a2 = G.sb("rp_a2", [64, C], stack=st)
        g2a = G.sb("rp_g2a", [128, C], stack=st)
        g2b = G.sb("rp_g2b", [32, C], stack=st)
        S.dma("sp", w2[:, :], W["w2"], writes=[w2])
        S.dma("sp", a2[:, :], W["a2"], writes=[a2])
        S.dma("sp", g2a[:, :], W["g2"][0:128, :], writes=[g2a])
        S.dma("sp", g2b[:, :], W["g2"][128:160, :], writes=[g2b])
        if vr:
            zv = G.sb("rp_zv", [32, T], stack=st)
            v2 = G.sb("rp_v2", [32, C], stack=st)
            S.dma("sp", zv[:, :], G.zf[3360:3392, :], writes=[zv])
            S.dma("sp", v2[:, :], W["v2"], writes=[v2])

        def mk(name, n=2, dt=F32, shape=(128, 512)):
            return [G.sb("rp_%s%d" % (name, i), list(shape), dt, stack=st) for i in range(n)]

        rt, kt, vt = mk("r"), mk("k"), mk("v")
        vft = mk("vf")
        dec, alr, gg = mk("dec"), mk("alr"), mk("g")
        t1, t2, t3 = mk("t1"), mk("t2"), mk("t3")
        arec, brec, kmod, bon = mk("arec"), mk("brec"), mk("kmod"), mk("bon")
        vtb = mk("vtb", 2, BF16, (128, 4, 128))
        pA = [G.ps("rp_pA%d" % i, [128, 512], stack=st) for i in range(2)]
        pB = [G.ps("rp_pB%d" % i, [128, 512], stack=st) for i in range(2)]
        pC = [G.ps("rp_pC%d" % i, [128, 512], stack=st) for i in range(2)]
        vtmv = G.vtm.rearrange("(s p) c -> p s c", p=128)
        it = 0
        for c in range(8):
            cs_ = slice(c * 128, (c + 1) * 128)
            for q in range(4):
                i = it % 2
                it += 1
                ts_ = slice(q * 512, (q + 1) * 512)
                r_, k_, v_ = rt[i], kt[i], vt[i]
                S.dma("sp", r_[:, :], G.zf[c * 128:(c + 1) * 128, ts_], writes=[r_])
                S.dma("sp", k_[:, :], G.zf[1024 + c * 128:1024 + (c + 1) * 128, ts_], writes=[k_])
                S.dma("sp", v_[:, :], G.zf[2048 + c * 128:2048 + (c + 1) * 128, ts_], writes=[v_])
                pa, pb, pc = pA[i], pB[i], pC[i]
                S.op("pe", lambda e, pa=pa: e.matmul(pa[:, :], w2[:, cs_], tzw[:, ts_], start=True, stop=True), reads=[w2, tzw], writes=[pa])
                d_ = dec[i]
                S.op("act", lambda e, pa=pa, d_=d_: e.activation(out=d_[:, :], in_=pa[:, :], func=AF.Sigmoid, bias=V(L, "w0", c)), reads=[pa, L.vec], writes=[d_])
                S.op("act", lambda e, d_=d_: e.activation(out=d_[:, :], in_=d_[:, :], func=AF.Exp, scale=-0.6065306597126334), reads=[d_], writes=[d_])
                S.dma("sp", G.rw["w"][cs_, ts_], d_[:, :], reads=[d_])
                S.op("pe", lambda e, pb=pb: e.matmul(pb[:, :], a2[:, cs_], za[:, ts_], start=True, stop=True), reads=[a2, za], writes=[pb])
                al = alr[i]
                S.op("act", lambda e, pb=pb, al=al: e.activation(out=al[:, :], in_=pb[:, :], func=AF.Sigmoid, bias=V(L, "a0", c)), reads=[pb, L.vec], writes=[al])
                S.op("pe", lambda e, pc=pc: e.matmul(pc[:, :], g2a[:, cs_], sga[:, ts_], start=True, stop=False), reads=[g2a, sga], writes=[pc])
                S.op("pe", lambda e, pc=pc: e.matmul(pc[:, :], g2b[:, cs_], sgb[:, ts_], start=False, stop=True), reads=[g2b, sgb], writes=[pc])
                g_ = gg[i]
                S.op("act", lambda e, pc=pc, g_=g_: e.copy(out=g_[:, :], in_=pc[:, :]), reads=[pc], writes=[g_])
                S.dma("sp", G.rw["g"][cs_, ts_], g_[:, :], reads=[g_])
                if vr:
                    vf = vft[i]
                    S.dma("sp", vf[:, :], G.vfirst[cs_, ts_], writes=[vf])
                    S.op("pe", lambda e, pa=pa: e.matmul(pa[:, :], v2[:, cs_], zv[:, ts_], start=True, stop=True), reads=[v2, zv], writes=[pa])
                    sv = t1[i]
                    S.op("act", lambda e, pa=pa, sv=sv: e.activation(out=sv[:, :], in_=pa[:, :], func=AF.Sigmoid, bias=V(L, "v0", c)), reads=[pa, L.vec], writes=[sv])
                    S.op("dve", lambda e, vf=vf, v_=v_: e.tensor_tensor(out=vf[:, :], in0=vf[:, :], in1=v_[:, :], op=ALU.subtract), reads=[vf, v_], writes=[vf])
                    S.op("dve", lambda e, vf=vf, sv=sv: e.tensor_tensor(out=vf[:, :], in0=vf[:, :], in1=sv[:, :], op=ALU.mult), reads=[vf, sv], writes=[vf])
                    S.op("dve", lambda e, vf=vf, v_=v_: e.tensor_tensor(out=v_[:, :], in0=v_[:, :], in1=vf[:, :], op=ALU.add), reads=[vf, v_], writes=[v_])
                else:
                    S.dma("sp", G.vfirst[cs_, ts_], v_[:, :], reads=[v_])
                S.dma("sp", G.rw["v"][cs_, ts_], v_[:, :], reads=[v_])
                kk = t2[i]
                sq = t3[i]
                S.op("dve", lambda e, kk=kk, k_=k_: e.tensor_scalar(out=kk[:, :], in0=k_[:, :], scalar1=V(L, "k_k", c), scalar2=None, op0=ALU.mult),
                     reads=[k_, L.vec], writes=[kk])
                S.op("act", lambda e, kk=kk, sq=sq: e.activation(out=sq[:, :], in_=kk[:, :], func=AF.Square), reads=[kk], writes=[sq])
                S.op("pe", lambda e, pb=pb, sq=sq: e.matmul(pb[:, :], G.bones, sq[:, :], start=True, stop=True), reads=[sq, G.cst], writes=[pb])
                S.op("dve", lambda e, pb=pb, sq=sq: e.tensor_scalar(out=sq[:, :], in0=pb[:, :], scalar1=1e-24, scalar2=None, op0=ALU.max),
                     reads=[pb], writes=[sq])
                S.op("act", lambda e, sq=sq: e.activation(out=sq[:, :], in_=sq[:, :], func=AF.Sqrt), reads=[sq], writes=[sq])
                S.op("dve", lambda e, sq=sq: e.reciprocal(out=sq[:, :], in_=sq[:, :]), reads=[sq], writes=[sq])
                S.op("dve", lambda e, kk=kk, sq=sq: e.tensor_tensor(out=kk[:, :], in0=kk[:, :], in1=sq[:, :], op=ALU.mult), reads=[kk, sq], writes=[kk])
                ar, br = arec[i], brec[i]
                S.op("act", lambda e, ar=ar, kk=kk: e.mul(out=ar[:, :], in_=kk[:, :], mul=-1.0), reads=[kk], writes=[ar])
                S.dma("sp", G.rw["a"][cs_, ts_], ar[:, :], reads=[ar])
                S.op("dve", lambda e, br=br, kk=kk, al=al: e.tensor_tensor(out=br[:, :], in0=kk[:, :], in1=al[:, :], op=ALU.mult), reads=[kk, al], writes=[br])
                S.dma("sp", G.rw["b"][cs_, ts_], br[:, :], reads=[br])
                km = kmod[i]
                S.op("dve", lambda e, al=al, km=km: e.tensor_scalar(out=km[:, :], in0=al[:, :], scalar1=-1.0, scalar2=V(L, "k_a", c), op0=ALU.add, op1=ALU.mult),
                     reads=[al, L.vec], writes=[km])
                S.op("dve", lambda e, km=km, k_=k_: e.scalar_tensor_tensor(out=km[:, :], in0=km[:, :], scalar=1.0, in1=k_[:, :], op0=ALU.add, op1=ALU.mult),
                     reads=[km, k_], writes=[km])
                S.dma("sp", G.rw["kmod"][cs_, ts_], km[:, :], reads=[km])
                bo_ = bon[i]
                S.op("dve", lambda e, bo_=bo_, r_=r_, km=km: e.scalar_tensor_tensor(out=bo_[:, :], in0=r_[:, :], scalar=V(L, "r_k", c), in1=km[:, :],
                                                                                   op0=ALU.mult, op1=ALU.mult), reads=[r_, km, L.vec], writes=[bo_])
                S.op("pe", lambda e, pc=pc, bo_=bo_: e.matmul(pc[:, :], G.bones, bo_[:, :], start=True, stop=True), reads=[bo_, G.cst], writes=[pc])
                S.op("dve", lambda e, pc=pc, bo_=bo_, v_=v_: e.tensor_tensor(out=bo_[:, :], in0=pc[:, :], in1=v_[:, :], op=ALU.mult), reads=[pc, v_], writes=[bo_])
                S.dma("sp", G.rw["bonus"][cs_, ts_], bo_[:, :], reads=[bo_])
                vb = vtb[i]
                for s4 in range(4):
                    S.op("pe", lambda e, pa=pa, v_=v_, s4=s4: e.transpose(out=pa[:, s4 * 128:(s4 + 1) * 128], in_=v_[:, s4 * 128:(s4 + 1) * 128], identity=G.ident),
                         reads=[v_, G.cst], writes=[pa])
                S.op("act", lambda e, pa=pa, vb=vb: e.copy(out=vb[:, :, :], in_=pa[:, :].rearrange("p (s c) -> p s c", s=4)), reads=[pa], writes=[vb])
                S.dma("sp", vtmv[:, q * 4:(q + 1) * 4, cs_], vb[:, :, :], reads=[vb])
        S.barrier()


def phase_rec(G, L):
    import os
    S, nc = G.S, G.nc
    TC = 128
    NS = 4
    with ExitStack() as st:
        St = G.sb("rc_S", [128, 2, 8, 64], stack=st)
        Sb = [[Buf() for _ in range(8)] for _ in range(2)]
        S.op("dve", lambda e: e.memset(St[:, :, :, :], 0.0), writes=[b for r in Sb for b in r])
        names = ("w", "b", "a", "r", "kmod")
        inb = {n: [G.sb("rc_%s%d" % (n, i), [128, 8, TC], stack=st) for i in range(2)] for n in names}
        vtb = [G.sb("rc_v%d" % i, [128, C], BF16, stack=st) for i in range(2)]
        tmpk = G.sb("rc_tmp", [128, 8, 64], stack=st)
        tmpb = [Buf() for _ in range(8)]
        pset = [G.ps("rc_ps%d" % i, [128, 2, 2, 64], stack=st) for i in range(NS)]
        psy = [G.ps("rc_psy%d" % i, [128, 8, 64], stack=st) for i in range(2)]
        ysb = [G.sb("rc_y%d" % i, [128, 8, TC], stack=st) for i in range(2)]
        yview = G.rw["y"].rearrange("(g p) t -> p g t", p=128)
        for ch in range(int(os.environ.get("REC_CHUNKS", T // TC))):
            t0 = ch * TC
            ib = {n: inb[n][ch % 2] for n in names}
            vb = vtb[ch % 2]
            for n in names:
                src = G.zf[0:C, :] if n == "r" else G.rw[n]
                S.dma("sp", ib[n][:, :, :], src.rearrange("(g p) t -> p g t", p=128)[:, :, t0:t0 + TC], writes=[ib[n]])
            S.dma("sp", vb[:, :], G.vtm[t0:t0 + TC, :], writes=[vb])
            ys = ysb[ch % 2]
            for tl in range(TC):
                t = t0 + tl
                o, n_ = t % 2, (t + 1) % 2
                py = psy[(tl // 64) % 2]
                yc = tl % 64
                for s_ in range(NS):
                    pb_ = pset[s_]
                    for gi in range(2):
                        g = s_ * 2 + gi
                        for h2 in range(2):
                            p0, p1 = h2 * 64, h2 * 64 + 64
                            kw = {"tile_position": (64, 64)} if h2 else {}
                            S.op("pe", lambda e, g=g, gi=gi, pb_=pb_, p0=p0, p1=p1, kw=kw, tl=tl, o=o, ib=ib: e.matmul(
                                pb_[p0:p1, gi, 0, :], ib["a"][p0:p1, g, tl:tl + 1].to_broadcast([64, 64]), St[p0:p1, o, g, :], start=True, stop=True, **kw),
                                reads=[Sb[o][g], ib["a"]], writes=[pb_])
                        for h2 in range(2):
                            p0, p1 = h2 * 64, h2 * 64 + 64
                            kw = {"tile_position": (0, 64)} if h2 else {}
                            hc = (g * 2 + h2) * 64
                            S.op("pe", lambda e, gi=gi, pb_=pb_, p0=p0, p1=p1, kw=kw, tl=tl, vb=vb, hc=hc: e.matmul(
                                pb_[p0:p1, gi, 1, :], G.identb[:, tl:tl + 1].to_broadcast([128, 64]), vb[:, hc:hc + 64], start=True, stop=True, **kw),
                                reads=[vb, G.cstb], writes=[pb_])
                for s_ in range(NS):
                    pb_ = pset[s_]
                    for gi in range(2):
                        g = s_ * 2 + gi
                        S.op("act", lambda e, g=g, gi=gi, pb_=pb_, tl=tl, ib=ib: e.activation(out=tmpk[:, g, :], in_=pb_[:, gi, 1, :], func=AF.Copy,
                                                                                           scale=ib["kmod"][:, g, tl:tl + 1]),
                             reads=[pb_, ib["kmod"]], writes=[tmpb[g]])
                    for gi in range(2):
                        g = s_ * 2 + gi
                        S.op("dve", lambda e, g=g, o=o, n_=n_, tl=tl, ib=ib: e.scalar_tensor_tensor(
                            out=St[:, n_, g, :], in0=St[:, o, g, :], scalar=ib["w"][:, g, tl:tl + 1], in1=tmpk[:, g, :], op0=ALU.mult, op1=ALU.add),
                            reads=[Sb[o][g], ib["w"], tmpb[g]], writes=[Sb[n_][g]])
                        S.op("dve", lambda e, g=g, gi=gi, pb_=pb_, n_=n_, tl=tl, ib=ib: e.scalar_tensor_tensor(
                            out=St[:, n_, g, :], in0=pb_[:, gi, 0, :], scalar=ib["b"][:, g, tl:tl + 1], in1=St[:, n_, g, :], op0=ALU.mult, op1=ALU.add),
                            reads=[pb_, ib["b"], Sb[n_][g]], writes=[Sb[n_][g]])
                for g in range(8):
                    for h2 in range(2):
                        p0, p1 = h2 * 64, h2 * 64 + 64
                        kw = {"tile_position": (64, 64)} if h2 else {}
                        S.op("pe", lambda e, g=g, p0=p0, p1=p1, kw=kw, tl=tl, n_=n_, py=py, yc=yc, ib=ib: e.matmul(
                            py[p0:p1, g, yc:yc + 1], St[p0:p1, n_, g, :], ib["r"][p0:p1, g, tl:tl + 1], start=True, stop=True, **kw),
                            reads=[Sb[n_][g], ib["r"]], writes=[py])
                if yc == 63:
                    half = tl // 64
                    S.op("act", lambda e, py=py, ys=ys, half=half: e.copy(out=ys[:, :, half * 64:(half + 1) * 64], in_=py[:, :, :]), reads=[py], writes=[ys])
            S.dma("sp", yview[:, :, t0:t0 + TC], ys[:, :, :], reads=[ys])
        S.barrier()


def phase_rwkv_post(G, L):
    S, nc = G.S, G.nc
    with ExitStack() as st:
        def mk(name, n=2, dt=F32, shape=(128, 512)):
            return [G.sb("rq_%s%d" % (name, i), list(shape), dt, stack=st) for i in range(n)]
        yt, bt, gt, dt_, sq, ob = mk("y"), mk("b"), mk("g"), mk("d"), mk("sq"), mk("ob", 2, BF16)
        pA = [G.ps("rq_pA%d" % i, [128, 512], stack=st) for i in range(2)]
        pB = [G.ps("rq_pB%d" % i, [128, 512], stack=st) for i in range(2)]
        it = 0
        for c in range(8):
            cs_ = slice(c * 128, (c + 1) * 128)
            for q in range(4):
                i = it % 2
                it += 1
                ts_ = slice(q * 512, (q + 1) * 512)
                y_, b_, g_, d_, s_, o_ = yt[i], bt[i], gt[i], dt_[i], sq[i], ob[i]
                pa, pb = pA[i], pB[i]
                S.dma("sp", y_[:, :], G.rw["y"][cs_, ts_], writes=[y_])
                S.dma("sp", b_[:, :], G.rw["bonus"][cs_, ts_], writes=[b_])
                S.dma("sp", g_[:, :], G.rw["g"][cs_, ts_], writes=[g_])
                S.op("pe", lambda e, pa=pa, y_=y_: e.matmul(pa[:, :], G.bones, y_[:, :], start=True, stop=True), reads=[y_, G.cst], writes=[pa])
                S.op("dve", lambda e, pa=pa, y_=y_, d_=d_: e.scalar_tensor_tensor(out=d_[:, :], in0=pa[:, :], scalar=-1.0 / 64, in1=y_[:, :], op0=ALU.mult, op1=ALU.add),
                     reads=[pa, y_], writes=[d_])
                S.op("act", lambda e, d_=d_, s_=s_: e.activation(out=s_[:, :], in_=d_[:, :], func=AF.Square), reads=[d_], writes=[s_])
                S.op("pe", lambda e, pb=pb, s_=s_: e.matmul(pb[:, :], G.bones, s_[:, :], start=True, stop=True), reads=[s_, G.cst], writes=[pb])
                S.op("act", lambda e, pb=pb, s_=s_: e.activation(out=s_[:, :], in_=pb[:, :], func=AF.Sqrt, scale=1.0 / 64, bias=G.cst.t[:, CO_EPSGN:CO_EPSGN + 1]),
                     reads=[pb, G.cst], writes=[s_])
                S.op("dve", lambda e, s_=s_: e.reciprocal(out=s_[:, :], in_=s_[:, :]), reads=[s_], writes=[s_])
                S.op("dve", lambda e, s_=s_, d_=d_: e.tensor_tensor(out=d_[:, :], in0=d_[:, :], in1=s_[:, :], op=ALU.mult), reads=[s_, d_], writes=[d_])
                S.op("dve", lambda e, d_=d_: e.tensor_scalar(out=d_[:, :], in0=d_[:, :], scalar1=V(L, "ln_w", c), scalar2=V(L, "ln_b", c), op0=ALU.mult, op1=ALU.add),
                     reads=[d_, L.vec], writes=[d_])
                S.op("dve", lambda e, d_=d_, b_=b_: e.tensor_tensor(out=d_[:, :], in0=d_[:, :], in1=b_[:, :], op=ALU.add), reads=[d_, b_], writes=[d_])
                S.op("dve", lambda e, d_=d_, g_=g_, o_=o_: e.tensor_tensor(out=o_[:, :], in0=d_[:, :], in1=g_[:, :], op=ALU.mult), reads=[d_, g_], writes=[o_])
                S.dma("sp", G.mixT[cs_, ts_], o_[:, :], reads=[o_])
        S.barrier()


def phase_moba(G, L):
    S, nc = G.S, G.nc
    l = L.l
    with ExitStack() as st:
        qf = [G.sb("mb_qf%d" % i, [128, T], stack=st) for i in range(2)]
        kf = [G.sb("mb_kf%d" % i, [128, T], stack=st) for i in range(2)]
        vf = [G.sb("mb_vf%d" % i, [128, T], stack=st) for i in range(2)]
        qb = G.sb("mb_qb", [128, T], BF16, stack=st)
        kb = G.sb("mb_kb", [128, T], BF16, stack=st)
        sqt = G.sb("mb_sq", [128, T], stack=st)
        km = G.sb("mb_km", [128, NB], stack=st)
        RBf = G.sb("mb_RBf", [33, T], stack=st)
        RB = G.sb("mb_RB", [33, T], BF16, stack=st)
        rowt = G.sb("mb_row", [33, T], stack=st)
        kx = G.sb("mb_kx", [33, 2], stack=st)
        va = G.sb("mb_va", [128, 16, 132], BF16, stack=st)
        gm = G.sb("mb_gm", [128, 8], stack=st)
        t8 = G.sb("mb_t8", [128, 8], stack=st)
        Et = [G.sb("mb_E%d" % i, [128, 256], BF16, stack=st) for i in range(2)]
        ot = G.sb("mb_o", [128, 128], stack=st)
        osq = G.sb("mb_osq", [128, 128], stack=st)
        osm = G.sb("mb_osm", [128, 4], stack=st)
        ymT = G.sb("mb_ymT", [128, T], BF16, stack=st)
        gain = G.sb("mb_gain", [128, 1024], stack=st)
        S.dma("sp", gain[:, :], G.rowp[:, RO_GAIN[l]:RO_GAIN[l] + 1024], writes=[gain])
        pq = G.ps("mb_pq", [33, 512], stack=st)
        pg = G.ps("mb_pg", [128, 8], stack=st)
        pt8 = G.ps("mb_pt8", [8, 128], stack=st)
        ptr = G.ps("mb_ptr", [128, 128], stack=st)
        pss = [G.ps("mb_ps%d" % i, [128, 256], stack=st) for i in range(2)]
        pso = [G.ps("mb_po%d" % i, [128, 132], stack=st) for i in range(2)]
        S.op("dve", lambda e: e.memset(RBf[:, :], 0.0), writes=[RBf])
        S.op("dve", lambda e: e.memset(va[:, :, 128:129], 1.0), writes=[va])
        one32 = G.cst.t[:, CO_ONE32:CO_ONE32 + 33]
        for h in range(8):
            q_, k_, v_ = qf[h % 2], kf[h % 2], vf[h % 2]
            S.dma("sp", q_[:, :], G.zf[ZQ + h * 128:ZQ + (h + 1) * 128, :], writes=[q_])
            S.dma("sp", k_[:, :], G.zf[ZQ + 1024 + h * 128:ZQ + 1024 + (h + 1) * 128, :], writes=[k_])
            S.dma("sp", v_[:, :], G.zf[ZQ + 2048 + h * 128:ZQ + 2048 + (h + 1) * 128, :], writes=[v_])
            S.op("act", lambda e, q_=q_: e.copy(out=qb[:, :], in_=q_[:, :]), reads=[q_], writes=[qb])
            S.op("act", lambda e, k_=k_: e.copy(out=kb[:, :], in_=k_[:, :]), reads=[k_], writes=[kb])
            S.op("dve", lambda e, k_=k_: e.tensor_reduce(out=km[:, :], in_=k_[:, :].rearrange("p (n s) -> p n s", n=NB), axis=AX.X, op=ALU.add),
                 reads=[k_], writes=[km])
            S.op("dve", lambda e: e.tensor_scalar(out=km[:, :], in0=km[:, :], scalar1=1.0 / 256, scalar2=None, op0=ALU.mult), reads=[km], writes=[km])
            S.op("act", lambda e, k_=k_: e.activation(out=sqt[:, :], in_=k_[:, :], func=AF.Square), reads=[k_], writes=[sqt])
            for q4 in range(4):
                S.op("pe", lambda e, q4=q4: e.matmul(pq[:, :], one32, sqt[:, q4 * 512:(q4 + 1) * 512], start=True, stop=True), reads=[sqt, G.cst], writes=[pq])
                S.op("dve", lambda e, q4=q4: e.tensor_copy(out=rowt[32:33, q4 * 512:(q4 + 1) * 512], in_=pq[32:33, :]), reads=[pq], writes=[rowt])
            S.op("dve", lambda e: e.tensor_reduce(out=kx[32:33, 0:1], in_=rowt[32:33, :], axis=AX.X, op=ALU.max), reads=[rowt], writes=[kx])
            S.op("act", lambda e, q_=q_: e.activation(out=sqt[:, :], in_=q_[:, :], func=AF.Square), reads=[q_], writes=[sqt])
            coef = -SLOPES[h] / SCALE
            for q4 in range(4):
                S.op("pe", lambda e, q4=q4: e.matmul(pq[:, :], one32, sqt[:, q4 * 512:(q4 + 1) * 512], start=True, stop=True), reads=[sqt, G.cst], writes=[pq])
                S.op("act", lambda e, q4=q4: e.activation(out=rowt[32:33, q4 * 512:(q4 + 1) * 512], in_=pq[32:33, :], func=AF.Sqrt, scale=kx[32:33, 0:1]),
                     reads=[pq, kx], writes=[rowt])
            S.op("dve", lambda e: e.scalar_tensor_tensor(out=RBf[32:33, :], in0=G.cst.t[32:33, CO_TROW:CO_TROW + T], scalar=coef, in1=rowt[32:33, :],
                                                         op0=ALU.mult, op1=ALU.subtract), reads=[rowt, G.cst], writes=[RBf])
            for tt in range(NT):
                own = tt // 2
                S.op("pe", lambda e, tt=tt, q_=q_: e.matmul(pg[:, :], q_[:, tt * 128:(tt + 1) * 128], km[:, :], start=True, stop=True), reads=[q_, km], writes=[pg])
                S.op("dve", lambda e, own=own: e.tensor_tensor(out=gm[:, :], in0=pg[:, :], in1=G.cst.t[:, CO_PAST + own * 8:CO_PAST + own * 8 + 8], op=ALU.add),
                     reads=[pg, G.cst], writes=[gm])
                S.op("dve", lambda e: e.max(out=t8[:, :], in_=gm[:, :]), reads=[gm], writes=[t8])
                S.op("dve", lambda e: e.tensor_scalar(out=gm[:, :], in0=gm[:, :], scalar1=t8[:, 2:3], scalar2=None, op0=ALU.is_ge), reads=[gm, t8], writes=[gm])
                S.op("dve", lambda e: e.tensor_scalar(out=gm[:, :], in0=gm[:, :], scalar1=-1.0, scalar2=BIGR, op0=ALU.add, op1=ALU.mult), reads=[gm], writes=[gm])
                S.op("pe", lambda e: e.transpose(out=pt8[:, :], in_=gm[:, :], identity=G.ident), reads=[gm, G.cst], writes=[pt8])
                S.op("act", lambda e, tt=tt: e.copy(out=RBf[0:8, tt * 128:(tt + 1) * 128], in_=pt8[:, :]), reads=[pt8], writes=[RBf])
            S.op("act", lambda e: e.copy(out=RB[:, :], in_=RBf[:, :]), reads=[RBf], writes=[RB])
            for stl in range(NT):
                S.op("pe", lambda e, stl=stl, v_=v_: e.transpose(out=ptr[:, :], in_=v_[:, stl * 128:(stl + 1) * 128], identity=G.ident), reads=[v_, G.cst], writes=[ptr])
                S.op("act", lambda e, stl=stl: e.copy(out=va[:, stl, 0:128], in_=ptr[:, :]), reads=[ptr], writes=[va])
            ei = 0
            for qblk in range(NB):
                tq = slice(qblk * 256, (qblk + 1) * 256)
                for n in range(qblk + 1):
                    for kt in range(2):
                        stl = 2 * n + kt
                        p_ = pss[ei % 2]
                        E_ = Et[ei % 2]
                        ei += 1
                        own = (n == qblk)
                        S.op("pe", lambda e, p_=p_, stl=stl, tq=tq: e.matmul(p_[:, :], kb[:, stl * 128:(stl + 1) * 128], qb[:, tq], start=True, stop=False),
                             reads=[kb, qb], writes=[p_])
                        sc = CO_SELC + (8 if own else n)
                        S.op("pe", lambda e, p_=p_, sc=sc, tq=tq, own=own: e.matmul(p_[:, :], G.cstb.t[0:33, sc:sc + 1].to_broadcast([33, 128]), RB[:, tq],
                                                                                  start=False, stop=(not own)), reads=[RB, G.cstb], writes=[p_])
                        if own:
                            cm = CO_CM1 if kt else CO_CM0
                            S.op("pe", lambda e, p_=p_, cm=cm: e.matmul(p_[:, :], G.identb, G.cstb.t[:, cm:cm + 256], start=False, stop=True),
                                 reads=[G.cstb], writes=[p_])
                        cb = CO_COLB + h * 16 + stl
                        S.op("act", lambda e, p_=p_, E_=E_, cb=cb: e.activation(out=E_[:, :], in_=p_[:, :], func=AF.Exp, scale=SCALE, bias=G.cst.t[:, cb:cb + 1]),
                             reads=[p_, G.cst], writes=[E_])
                        for tsub in range(2):
                            if own and kt == 1 and tsub == 0:
                                continue
                            first = (n == 0 and kt == 0)
                            last = own and (kt == tsub)
                            S.op("pe", lambda e, tsub=tsub, E_=E_, stl=stl, first=first, last=last: e.matmul(
                                pso[tsub][:, 0:129], E_[:, tsub * 128:(tsub + 1) * 128], va[:, stl, 0:129], start=first, stop=last),
                                reads=[E_, va], writes=[pso[tsub]])
                for tsub in range(2):
                    po = pso[tsub]
                    tcol = qblk * 256 + tsub * 128
                    S.op("dve", lambda e, po=po: e.reciprocal(out=osm[:, 0:1], in_=po[:, 128:129]), reads=[po], writes=[osm])
                    S.op("act", lambda e, po=po: e.activation(out=ot[:, :], in_=po[:, 0:128], func=AF.Copy, scale=osm[:, 0:1]), reads=[po, osm], writes=[ot])
                    S.op("act", lambda e: e.activation(out=osq[:, :], in_=ot[:, :], func=AF.Square), reads=[ot], writes=[osq])
                    S.op("dve", lambda e: e.tensor_reduce(out=osm[:, 1:2], in_=osq[:, :], axis=AX.X, op=ALU.add), reads=[osq], writes=[osm])
                    S.op("act", lambda e: e.activation(out=osm[:, 2:3], in_=osm[:, 1:2], func=AF.Sqrt, scale=1.0 / 128, bias=G.cst.t[:, CO_EPS6:CO_EPS6 + 1]),
                         reads=[osm, G.cst], writes=[osm])
                    S.op("dve", lambda e: e.reciprocal(out=osm[:, 3:4], in_=osm[:, 2:3]), reads=[osm], writes=[osm])
                    S.op("dve", lambda e: e.scalar_tensor_tensor(out=ot[:, :], in0=ot[:, :], scalar=osm[:, 3:4], in1=gain[:, h * 128:(h + 1) * 128],
                                                                 op0=ALU.mult, op1=ALU.mult), reads=[ot, osm, gain], writes=[ot])
                    S.op("pe", lambda e: e.transpose(out=ptr[:, :], in_=ot[:, :], identity=G.ident), reads=[ot, G.cst], writes=[ptr])
                    S.op("act", lambda e, tcol=tcol: e.copy(out=ymT[:, tcol:tcol + 128], in_=ptr[:, :]), reads=[ptr], writes=[ymT])
            S.dma("sp", G.mixT[C + h * 128:C + (h + 1) * 128, :], ymT[:, :], reads=[ymT])
        S.barrier()


def bcast_rows(G, st, src16, name):
    S = G.S
    o = G.sb(name, [128, D], stack=st)
    tr = G.sb(name + "_tr", [16, 128], stack=st)
    p1 = G.ps(name + "_p1", [16, 128], stack=st)
    p2 = G.ps(name + "_p2", [128, 512], stack=st)
    src, srcb = src16
    S.op("pe", lambda e: e.transpose(out=p1[:, :], in_=src, identity=G.ident), reads=[srcb, G.cst], writes=[p1])
    S.op("act", lambda e: e.copy(out=tr[:, :], in_=p1[:, :]), reads=[p1], writes=[tr])
    for q in range(4):
        for kk in range(4):
            k = q * 4 + kk
            S.op("pe", lambda e, k=k, kk=kk: e.matmul(p2[:, kk * 128:(kk + 1) * 128], G.ident[0:16, k:k + 1].to_broadcast([16, 128]), tr[:, :],
                                                     start=True, stop=True), reads=[tr, G.cst], writes=[p2])
        S.op("act", lambda e, q=q: e.copy(out=o[:, q * 512:(q + 1) * 512], in_=p2[:, :]), reads=[p2], writes=[o])
    return o


def phase_wout(G, L, x_dram):
    S, nc, W = G.S, G.nc, L.W
    with ExitStack() as st:
        gbc = bcast_rows(G, st, (L.der.t[:, 32:48], L.der), "wo_gbc")
        wo = G.sb("wo_w", [128, 16, D], BF16, stack=st)
        wb = [Buf() for _ in range(4)]
        wv = W["w_out"].rearrange("(k p) n -> p k n", p=128)
        for q in range(4):
            S.dma("pool", wo[:, :, q * 512:(q + 1) * 512], wv[:, :, q * 512:(q + 1) * 512], writes=[wb[q]])
        mt = [G.sb("wo_m%d" % i, [128, 16, 128], BF16, stack=st) for i in range(2)]
        xt = [G.sb("wo_x%d" % i, [128, D], stack=st) for i in range(2)]
        xo = [G.sb("wo_o%d" % i, [128, D], stack=st) for i in range(2)]
        pp = [G.ps("wo_p%d" % i, [128, 512], stack=st) for i in range(2)]
        mview = G.mixT.rearrange("(k p) t -> p k t", p=128)
        for tt in range(NT):
            m_, x_, o_ = mt[tt % 2], xt[tt % 2], xo[tt % 2]
            S.dma("sp", m_[:, :, :], mview[:, :, tt * 128:(tt + 1) * 128], writes=[m_])
            S.dma("sp", x_[:, :], x_dram[tt * 128:(tt + 1) * 128, :], writes=[x_])
            for q in range(4):
                p_ = pp[q % 2]
                for k in range(16):
                    S.op("pe", lambda e, p_=p_, m_=m_, k=k, q=q: e.matmul(p_[:, :], m_[:, k, :], wo[:, k, q * 512:(q + 1) * 512], start=(k == 0), stop=(k == 15)),
                         reads=[m_, wb[q]], writes=[p_])
                S.op("dve", lambda e, p_=p_, o_=o_, q=q: e.tensor_tensor(out=o_[:, q * 512:(q + 1) * 512], in0=p_[:, :], in1=gbc[:, q * 512:(q + 1) * 512], op=ALU.mult),
                     reads=[p_, gbc], writes=[o_])
            S.op("pool", lambda e, o_=o_, x_=x_: e.tensor_tensor(out=o_[:, :], in0=o_[:, :], in1=x_[:, :], op=ALU.add), reads=[o_, x_], writes=[o_])
            S.dma("sp", G.xa[tt * 128:(tt + 1) * 128, :], o_[:, :], reads=[o_])
        S.barrier()


def phase_ffn(G, L):
    S, nc, W = G.S, G.nc, L.W
    moe = (L.l % 2 == 1)
    import os
    ne = int(os.environ.get('MOE_NE', NEXP)) if moe else 1
    with ExitStack() as st:
        hT = G.sb("ff_hT", [128, 16, 512], BF16, stack=st)
        acc = G.sb("ff_acc", [128, 16, 512], stack=st)
        act = G.sb("ff_act", [128, FC, 512], BF16, stack=st)
        actb = [Buf() for _ in range(FC)]
        wg = [G.sb("ff_wg%d" % i, [128, 16, 128], BF16, stack=st) for i in range(2)]
        wu = [G.sb("ff_wu%d" % i, [128, 16, 128], BF16, stack=st) for i in range(2)]
        wd = [G.sb("ff_wd%d" % i, [128, FC, 128], BF16, stack=st) for i in range(2)]
        sg = [G.sb("ff_sg%d" % i, [128, 512], stack=st) for i in range(2)]
        gbc = G.sb("ff_gbc", [128, 512], stack=st)
        xt = [G.sb("ff_x%d" % i, [128, D], stack=st) for i in range(2)]
        pg_ = [G.ps("ff_pg%d" % i, [128, 512], stack=st) for i in range(2)]
        pu_ = [G.ps("ff_pu%d" % i, [128, 512], stack=st) for i in range(2)]
        pd_ = [G.ps("ff_pd%d" % i, [128, 512], stack=st) for i in range(2)]
        if moe:
            gts = G.sb("ff_gts", [NEXP, T], stack=st)
            S.dma("sp", gts[:, :], G.gT, writes=[gts])
        hview = G.hT.rearrange("(k p) t -> p k t", p=128)
        wi = 0
        di = 0
        for q in range(4):
            ts_ = slice(q * 512, (q + 1) * 512)
            S.dma("sp", hT[:, :, :], hview[:, :, ts_], writes=[hT])
            for e_ in range(ne):
                if moe:
                    S.op("pe", lambda e, e_=e_, ts_=ts_: e.matmul(pd_[0][:, :], G.ident[0:NEXP, e_:e_ + 1].to_broadcast([NEXP, 128]), gts[:, ts_], start=True, stop=True),
                         reads=[gts, G.cst], writes=[pd_[0]])
                    S.op("act", lambda e: e.copy(out=gbc[:, :], in_=pd_[0][:, :]), reads=[pd_[0]], writes=[gbc])
                gv = W["ffn_gate"][e_].rearrange("(k p) f -> p k f", p=128)
                uv = W["ffn_up"][e_].rearrange("(k p) f -> p k f", p=128)
                dv = W["ffn_down"][e_].rearrange("(c p) d -> p c d", p=128)
                for fc in range(FC):
                    g_, u_ = wg[wi % 2], wu[wi % 2]
                    pg, pu, s_ = pg_[wi % 2], pu_[wi % 2], sg[wi % 2]
                    wi += 1
                    S.dma("pool", g_[:, :, :], gv[:, :, fc * 128:(fc + 1) * 128], writes=[g_])
                    S.dma("pool", u_[:, :, :], uv[:, :, fc * 128:(fc + 1) * 128], writes=[u_])
                    for k in range(16):
                        S.op("pe", lambda e, pg=pg, g_=g_, k=k: e.matmul(pg[:, :], g_[:, k, :], hT[:, k, :], start=(k == 0), stop=(k == 15)), reads=[g_, hT], writes=[pg])
                    for k in range(16):
                        S.op("pe", lambda e, pu=pu, u_=u_, k=k: e.matmul(pu[:, :], u_[:, k, :], hT[:, k, :], start=(k == 0), stop=(k == 15)), reads=[u_, hT], writes=[pu])
                    S.op("act", lambda e, pg=pg, s_=s_: e.activation(out=s_[:, :], in_=pg[:, :], func=AF.Silu), reads=[pg], writes=[s_])
                    if moe:
                        S.op("dve", lambda e, s_=s_: e.tensor_tensor(out=s_[:, :], in0=s_[:, :], in1=gbc[:, :], op=ALU.mult), reads=[s_, gbc], writes=[s_])
                    S.op("dve", lambda e, s_=s_, pu=pu, fc=fc: e.tensor_tensor(out=act[:, fc, :], in0=s_[:, :], in1=pu[:, :], op=ALU.mult), reads=[s_, pu], writes=[actb[fc]])
                for dc in range(16):
                    d_ = wd[di % 2]
                    pd = pd_[di % 2]
                    di += 1
                    S.dma("pool", d_[:, :, :], dv[:, :, dc * 128:(dc + 1) * 128], writes=[d_])
                    for fc in range(FC):
                        S.op("pe", lambda e, pd=pd, d_=d_, fc=fc: e.matmul(pd[:, :], d_[:, fc, :], act[:, fc, :], start=(fc == 0), stop=(fc == FC - 1)),
                             reads=[d_, actb[fc]], writes=[pd])
                    if e_ == 0:
                        S.op("act", lambda e, pd=pd, dc=dc: e.copy(out=acc[:, dc, :], in_=pd[:, :]), reads=[pd], writes=[acc])
                    else:
                        S.op("dve", lambda e, pd=pd, dc=dc: e.tensor_tensor(out=acc[:, dc, :], in0=acc[:, dc, :], in1=pd[:, :], op=ALU.add), reads=[pd, acc], writes=[acc])
            for dc in range(16):
                S.op("act", lambda e, dc=dc: e.activation(out=acc[:, dc, :], in_=acc[:, dc, :], func=AF.Copy, scale=L.der[:, 80 + dc:81 + dc]), reads=[acc, L.der], writes=[acc])
            for s4 in range(4):
                tt = q * 4 + s4
                x_ = xt[s4 % 2]
                S.dma("sp", x_[:, :], G.xa[tt * 128:(tt + 1) * 128, :], writes=[x_])
                for d4 in range(4):
                    p_ = pu_[d4 % 2]
                    for dd in range(4):
                        dc = d4 * 4 + dd
                        S.op("pe", lambda e, p_=p_, dd=dd, dc=dc, s4=s4: e.transpose(out=p_[:, dd * 128:(dd + 1) * 128], in_=acc[:, dc, s4 * 128:(s4 + 1) * 128], identity=G.ident),
                             reads=[acc, G.cst], writes=[p_])
                    S.op("dve", lambda e, p_=p_, x_=x_, d4=d4: e.tensor_tensor(out=x_[:, d4 * 512:(d4 + 1) * 512], in0=x_[:, d4 * 512:(d4 + 1) * 512], in1=p_[:, :], op=ALU.add),
                         reads=[p_, x_], writes=[x_])
                S.dma("sp", G.xb[tt * 128:(tt + 1) * 128, :], x_[:, :], reads=[x_])
        S.barrier()


def phase_final(G, x_dram):
    S, nc = G.S, G.nc
    with ExitStack() as st:
        xt = [G.sb("fn_x%d" % i, [128, D], stack=st) for i in range(2)]
        sq = G.sb("fn_sq", [128, D], stack=st)
        ss = [G.sb("fn_ss%d" % i, [128, 2], stack=st) for i in range(2)]
        gn = G.sb("fn_g", [128, D], stack=st)
        S.dma("sp", gn[:, :], G.rowp[:, RO_NOUT:RO_NOUT + D], writes=[gn])
        for tt in range(NT):
            x, s_ = xt[tt % 2], ss[tt % 2]
            S.dma("sp", x[:, :], x_dram[tt * 128:(tt + 1) * 128, :], writes=[x])
            S.op("act", lambda e, x=x: e.activation(out=sq[:, :], in_=x[:, :], func=AF.Square), reads=[x], writes=[sq])
            S.op("dve", lambda e, s_=s_: e.tensor_reduce(out=s_[:, 0:1], in_=sq[:, :], axis=AX.X, op=ALU.add), reads=[sq], writes=[s_])
            S.op("act", lambda e, s_=s_: e.activation(out=s_[:, 1:2], in_=s_[:, 0:1], func=AF.Sqrt, scale=1.0 / D, bias=G.cst.t[:, CO_EPS6:CO_EPS6 + 1]),
                 reads=[s_, G.cst], writes=[s_])
            S.op("dve", lambda e, s_=s_: e.reciprocal(out=s_[:, 0:1], in_=s_[:, 1:2]), reads=[s_], writes=[s_])
            S.op("dve", lambda e, x=x, s_=s_: e.scalar_tensor_tensor(out=x[:, :], in0=x[:, :], scalar=s_[:, 0:1], in1=gn[:, :], op0=ALU.mult, op1=ALU.mult),
                 reads=[x, s_, gn], writes=[x])
            S.dma("sp", G.out[tt * 128:(tt + 1) * 128, :], x[:, :], reads=[x])
        S.barrier()


def make_in_map(inp, b, n_layers=2):
    m = {"x": np.ascontiguousarray(inp["x"][b]), "consts": make_consts(), "rowp": make_rowp(inp)}
    for l in range(n_layers):
        p = "l%d_" % l
        m[p + "vecs"] = make_vecs(inp, l, b)
        for k in ("mod_w", "w_in", "w2", "a2", "g2", "w_out"):
            m[p + k] = np.ascontiguousarray(inp[p + k])
        if l > 0:
            m[p + "v2"] = np.ascontiguousarray(inp[p + "v2"])
        if l % 2 == 0:
            for k in ("ffn_gate", "ffn_up", "ffn_down"):
                m[p + k] = np.ascontiguousarray(inp[p + k])[None]
        else:
            import os
            nin = int(os.environ.get("DEV_NEXP", NEXP))
            m[p + "router"] = np.ascontiguousarray(inp[p + "router"].reshape(16, 128, NEXP).transpose(1, 0, 2).reshape(128, 16 * NEXP))
            for k in ("exp_gate", "exp_up", "exp_down"):
                m[p + k] = np.ascontiguousarray(inp[p + k][:nin])
    return m


def kernel(**inputs):
    inp = {k: np.asarray(v) for k, v in inputs.items()}
    nc = build_program()
    maps = [make_in_map(inp, b) for b in range(4)]
    in_maps = [maps[c % 4] for c in range(N_CORES)]
    res = run_bass_kernel_spmd(nc, in_maps, core_ids=list(range(N_CORES)))
    out = np.stack([np.asarray(res.results[b]["out"]) for b in range(4)], axis=0)
    return out.astype(np.float32)
```

```python
import numpy as np
from contextlib import ExitStack
import concourse.bass as bass
import concourse.mybir as mybir
from concourse.bass_utils import run_bass_kernel_spmd

F32 = mybir.dt.float32
BF16 = mybir.dt.bfloat16
AF = mybir.ActivationFunctionType
ALU = mybir.AluOpType
AX = mybir.AxisListType

T = 2048
D = 2048
NT = T // 128
DC = D // 128
C = 1024
DFF = 5632
FC = DFF // 128
NEXP = 8
NB = 8
SCALE = 128 ** -0.5
BIGR = 1.0e5
N_CORES = 4


class Buf:
    __slots__ = ("w", "r")

    def __init__(self):
        self.w = None
        self.r = {}


class Tile:
    def __init__(self, t, shape):
        self.t = t
        self.b = Buf()
        self.shape = shape

    def __getitem__(self, k):
        return self.t[k]


class Sched:
    LIMIT = 60000
    NDS = 24

    def __init__(self, nc, es):
        self.nc = nc
        self.es = es
        self.E = {"pe": nc.tensor, "act": nc.scalar, "dve": nc.vector, "pool": nc.gpsimd, "sp": nc.sync}
        self.semobj = {}
        self.cur = {}
        self.cnt = {}
        self.nsem = 0
        self.known = {e: {} for e in self.E}
        self.latest = {}
        for e in ("pe", "act", "dve", "pool"):
            self._newsem(e)
        self.dsem = {q: [self._alloc("d%s%d" % (q, i)) for i in range(self.NDS)] for q in ("sp", "pool")}
        self.duse = {q: [0] * self.NDS for q in ("sp", "pool")}
        self.dnext = {"sp": 0, "pool": 0}
        self.n_ins = 0

    def _alloc(self, name):
        s = self.es.enter_context(self.nc.semaphore(name))
        k = self.nsem
        self.nsem += 1
        self.semobj[k] = s
        return k

    def _newsem(self, e):
        k = self._alloc("%s_%d" % (e, self.nsem))
        self.cur[e] = k
        self.cnt[e] = 0

    def _wait(self, e, deps):
        kn = self.known[e]
        best = {}
        for (k, v) in deps:
            if best.get(k, 0) < v:
                best[k] = v
        for k, v in best.items():
            if e == "pe" and k == self.cur["pe"]:
                continue
            if kn.get(k, 0) >= v:
                continue
            self.E[e].wait_ge(self.semobj[k], v)
            kn[k] = v
            self.n_ins += 1

    def _deps(self, reads, writes):
        deps = []
        for b in reads:
            if b.w is not None:
                deps.append(b.w)
        for b in writes:
            if b.w is not None:
                deps.append(b.w)
            deps.extend(b.r.items())
        return deps

    def _mark(self, ev, reads, writes):
        for b in reads:
            if b.r.get(ev[0], 0) < ev[1]:
                b.r[ev[0]] = ev[1]
        for b in writes:
            b.w = ev
            b.r = {}
        self.latest[ev[0]] = ev[1]

    def op(self, e, fn, reads=(), writes=()):
        reads = [x.b if isinstance(x, Tile) else x for x in reads]
        writes = [x.b if isinstance(x, Tile) else x for x in writes]
        self._wait(e, self._deps(reads, writes))
        if self.cnt[e] >= self.LIMIT:
            self._newsem(e)
        ins = fn(self.E[e])
        k = self.cur[e]
        ins.then_inc(self.semobj[k], 1)
        self.cnt[e] += 1
        self.n_ins += 1
        self._mark((k, self.cnt[e]), reads, writes)

    def dma(self, q, out, in_, reads=(), writes=(), **kw):
        reads = [x.b if isinstance(x, Tile) else x for x in reads]
        writes = [x.b if isinstance(x, Tile) else x for x in writes]
        i = self.dnext[q]
        self.dnext[q] = (i + 1) % self.NDS
        dsem, duse = self.dsem[q], self.duse[q]
        deps = self._deps(reads, writes)
        if duse[i] > 0:
            deps.append((dsem[i], 16 * duse[i]))
        self._wait(q, deps)
        ins = self.E[q].dma_start(out=out, in_=in_, **kw)
        ins.then_inc(self.semobj[dsem[i]], 16)
        duse[i] += 1
        self.n_ins += 1
        self._mark((dsem[i], 16 * duse[i]), reads, writes)

    def barrier(self, engines=("pe", "act", "dve", "pool", "sp")):
        evs = list(self.latest.items())
        for e in engines:
            self._wait(e, evs)


class Ctx:
    pass


class StopBuild(Exception):
    pass


def build_program(n_layers=2, dbg=(), stop=None):
    nc = bass.Bass("TRN2", target_bir_lowering=False)
    es = ExitStack()
    S = Sched(nc, es)
    G = Ctx()
    G.nc, G.S, G.es = nc, S, es

    def din(name, shape, dt=F32):
        return nc.dram_tensor(name, list(shape), dt, kind="ExternalInput").ap()

    def dscr(name, shape, dt=F32):
        if name in dbg:
            return nc.dram_tensor(name, list(shape), dt, kind="ExternalOutput").ap()
        return nc.dram_tensor(name, list(shape), dt).ap()
    G.stop = stop

    G.x_in = din("x", [T, D])
    G.consts = din("consts", [128, NCONST])
    G.rowp = din("rowp", [128, NROW])
    G.out = nc.dram_tensor("out", [T, D], F32, kind="ExternalOutput").ap()
    G.W = []
    for l in range(n_layers):
        vr = l > 0
        ncols = 3360 + (32 if vr else 0) + 3072
        w = {}
        w["vecs"] = din("l%d_vecs" % l, [128, NVEC])
        w["mod_w"] = din("l%d_mod_w" % l, [D, 6 * D])
        w["w_in"] = din("l%d_w_in" % l, [D, ncols])
        w["w2"] = din("l%d_w2" % l, [64, C])
        w["a2"] = din("l%d_a2" % l, [64, C])
        w["g2"] = din("l%d_g2" % l, [160, C])
        if vr:
            w["v2"] = din("l%d_v2" % l, [32, C])
        w["w_out"] = din("l%d_w_out" % l, [D, D])
        if l % 2 == 0:
            w["ffn_gate"] = din("l%d_ffn_gate" % l, [1, D, DFF])
            w["ffn_up"] = din("l%d_ffn_up" % l, [1, D, DFF])
            w["ffn_down"] = din("l%d_ffn_down" % l, [1, DFF, D])
        else:
            import os
            nin = int(os.environ.get("DEV_NEXP", NEXP))
            w["router"] = din("l%d_router" % l, [128, 16 * NEXP])
            w["ffn_gate"] = din("l%d_exp_gate" % l, [nin, D, DFF])
            w["ffn_up"] = din("l%d_exp_up" % l, [nin, D, DFF])
            w["ffn_down"] = din("l%d_exp_down" % l, [nin, DFF, D])
        w["ncols"] = ncols
        w["vr"] = vr
        G.W.append(w)

    G.xa = dscr("xa", [T, D])
    G.xb = dscr("xb", [T, D])
    G.hT = dscr("hT", [D, T], BF16)
    G.zf = dscr("zf", [6464, T])
    G.rw = {k: dscr("rw_" + k, [C, T]) for k in ("w", "kmod", "v", "a", "b", "g", "bonus", "y")}
    G.vtm = dscr("vtm", [T, C], BF16)
    G.vfirst = dscr("vfirst", [C, T])
    G.mixT = dscr("mixT", [D, T], BF16)
    G.gT = dscr("gT", [NEXP, T])
    G.dbg = {}
    if "mod" in dbg:
        G.dbg["mod"] = nc.dram_tensor("dbg_mod", [128, 96], F32, kind="ExternalOutput").ap()

    G.uid = 0

    def sb(name, shape, dt=F32, stack=None):
        G.uid += 1
        name = "%s_u%d" % (name, G.uid)
        t = (stack or es).enter_context(nc.sbuf_tensor(name, list(shape), dt))
        return Tile(t, shape)

    def ps(name, shape, dt=F32, stack=None):
        G.uid += 1
        name = "%s_u%d" % (name, G.uid)
        full = (stack or es).enter_context(nc.psum_tensor(name, [128, 512], F32))
        shape = list(shape)
        n = 1
        for d_ in shape[1:]:
            n *= d_
        assert n <= 512 and dt == F32
        v = full[0:shape[0], 0:n]
        if len(shape) == 3:
            v = v.rearrange("p (a b) -> p a b", a=shape[1])
        elif len(shape) == 4:
            v = v.rearrange("p (a b c) -> p a b c", a=shape[1], b=shape[2])
        return Tile(v, shape)

    G.sb, G.ps = sb, ps
    G.cst = sb("cst", [128, NCONST])
    S.dma("sp", G.cst[:, :], G.consts, writes=[G.cst])
    G.ident = G.cst.t[:, CO_IDENT:CO_IDENT + 128]
    G.bones = G.cst.t[:, CO_BONES:CO_BONES + 128]
    G.cstb = sb("cstb", [128, NCONST], BF16)
    S.op("act", lambda e: e.copy(out=G.cstb[:, :], in_=G.cst[:, :]), reads=[G.cst], writes=[G.cstb])
    G.identb = G.cstb.t[:, CO_IDENT:CO_IDENT + 128]

    try:
        for l in range(n_layers):
            build_layer(G, l)
        phase_final(G, G.xb)
    except StopBuild:
        pass
    S.barrier()
    G.n_ins = S.n_ins
    build_program.last = G
    return nc


CO_IDENT = 0
CO_BONES = 128
CO_ONE32 = 256
CO_SELC = 289
CO_TROW = 305
CO_CM0 = CO_TROW + T
CO_CM1 = CO_CM0 + 256
CO_COLB = CO_CM1 + 256
CO_PAST = CO_COLB + 128
CO_M0 = CO_PAST + 64
CO_EPS6 = CO_M0 + 2
CO_EPSGN = CO_M0 + 3
NCONST = CO_M0 + 4


def make_consts():
    c = np.zeros((128, NCONST), np.float32)
    c[:, CO_IDENT:CO_IDENT + 128] = np.eye(128, dtype=np.float32)
    bo = np.zeros((128, 128), np.float32)
    bo[:64, :64] = 1
    bo[64:, 64:] = 1
    c[:, CO_BONES:CO_BONES + 128] = bo
    c[:, CO_ONE32 + 32] = 1.0
    for n in range(8):
        c[n, CO_SELC + n] = 1.0
        c[32, CO_SELC + n] = 1.0
    c[32, CO_SELC + 8] = 1.0
    c[32, CO_TROW:CO_TROW + T] = np.arange(T, dtype=np.float32)
    s = np.arange(128)[:, None]
    t = np.arange(256)[None, :]
    c[:, CO_CM0:CO_CM0 + 256] = np.where(t >= s, 0.0, -BIGR)
    c[:, CO_CM1:CO_CM1 + 256] = np.where(t >= s + 128, 0.0, -BIGR)
    slopes = np.exp2(-8.0 * np.arange(1, 9, dtype=np.float32) / 8).astype(np.float32)
    for h in range(8):
        for st in range(16):
            c[:, CO_COLB + h * 16 + st] = slopes[h] * (st * 128 + np.arange(128, dtype=np.float32))
    for own in range(8):
        for n in range(8):
            c[:, CO_PAST + own * 8 + n] = 0.0 if n < own else -1.0e30
    c[:64, CO_M0] = 1.0
    c[64:, CO_M0 + 1] = 1.0
    c[:, CO_EPS6] = 1e-6
    c[:, CO_EPSGN] = 64e-5
    return c


SLOPES = [float(2.0 ** (-8.0 * (h + 1) / 8)) for h in range(8)]

VO = {}
_o = 0
for _n, _k in (("c", 16), ("mod_b", 96), ("norm_mix", 16), ("norm_ffn", 16), ("mu_r", 8), ("mu_k", 8), ("mu_v", 8),
               ("mu_zw", 1), ("mu_za", 1), ("mu_zg", 2), ("mu_zv", 1), ("mu_zg2", 1), ("w0", 8), ("a0", 8), ("v0", 8), ("k_k", 8),
               ("k_a", 8), ("r_k", 8), ("ln_w", 8), ("ln_b", 8)):
    VO[_n] = _o
    _o += _k
NVEC = _o

RO_GAIN = [0, 1024]
RO_NOUT = 2048
NROW = 4096


def cols(v, k):
    v = np.asarray(v, np.float32).reshape(-1)
    o = np.zeros((128, k), np.float32)
    n = v.shape[0]
    full = n // 128
    if full:
        o[:, :full] = v[:full * 128].reshape(full, 128).T
    rem = n - full * 128
    if rem:
        o[:rem, full] = v[full * 128:]
    return o


def make_vecs(inp, l, b):
    p = "l%d_" % l
    v = np.zeros((128, NVEC), np.float32)

    def put(name, arr, k):
        v[:, VO[name]:VO[name] + k] = cols(arr, k)

    put("c", inp["c"][b], 16)
    put("mod_b", inp[p + "mod_b"], 96)
    put("norm_mix", inp[p + "norm_mix"], 16)
    put("norm_ffn", inp[p + "norm_ffn"], 16)
    mu = inp[p + "shift_mu"]
    put("mu_r", mu[0:1024], 8)
    put("mu_k", mu[1024:2048], 8)
    put("mu_v", mu[2048:3072], 8)
    put("mu_zw", mu[3072:3136], 1)
    put("mu_za", mu[3136:3200], 1)
    put("mu_zg", mu[3200:3328], 1)
    put("mu_zg2", mu[3328:3360], 1)
    if l > 0:
        put("mu_zv", mu[3360:3392], 1)
        put("v0", inp[p + "v0"], 8)
    put("w0", inp[p + "w0"], 8)
    put("a0", inp[p + "a0"], 8)
    put("k_k", inp[p + "k_k"], 8)
    put("k_a", inp[p + "k_a"], 8)
    put("r_k", inp[p + "r_k"].reshape(-1), 8)
    put("ln_w", inp[p + "ln_w"], 8)
    put("ln_b", inp[p + "ln_b"], 8)
    return v


def make_rowp(inp):
    r = np.zeros((128, NROW), np.float32)
    r[:, 0:1024] = np.broadcast_to(inp["l0_moba_gain"][None, :], (128, 1024))
    r[:, 1024:2048] = np.broadcast_to(inp["l1_moba_gain"][None, :], (128, 1024))
    r[:, 2048:4096] = np.broadcast_to(inp["norm_out"][None, :], (128, 2048))
    return r


def build_layer(G, l):
    S = G.S
    W = G.W[l]
    x_in = G.x_in if l == 0 else G.xb
    L = Ctx()
    L.l = l
    L.W = W
    S.barrier()
    with ExitStack() as ls:
        L.vec = G.sb("vec%d" % l, [128, NVEC], stack=ls)
        S.dma("sp", L.vec[:, :], W["vecs"], writes=[L.vec])
        L.modv = G.sb("modv%d" % l, [128, 96], stack=ls)
        L.der = G.sb("der%d" % l, [128, 96], stack=ls)
        L.om = G.sb("om%d" % l, [128, NVEC], stack=ls)
        seq = [("mod", lambda: phase_mod(G, L)),
               ("norm1", lambda: phase_norm(G, L, x_in, 0, 16, router=None)),
               ("proj", lambda: phase_proj(G, L)),
               ("pre", lambda: phase_rwkv_pre(G, L)),
               ("rec", lambda: phase_rec(G, L)),
               ("post", lambda: phase_rwkv_post(G, L)),
               ("moba", lambda: phase_moba(G, L)),
               ("wout", lambda: phase_wout(G, L, x_in)),
               ("norm2", lambda: phase_norm(G, L, G.xa, 48, 64, router=(W.get("router")))),
               ("ffn", lambda: phase_ffn(G, L))]
        skip = getattr(G, "skip", ())
        for name, fn in seq:
            if name not in skip and (l, name) not in skip:
                fn()
                S.barrier()
            if G.stop == (l, name):
                raise StopBuild()
        S.barrier()


def V(L, name, j=0, n=1, p0=0, p1=128):
    o = VO[name] + j
    return L.vec.t[p0:p1, o:o + n]


def phase_mod(G, L):
    S, nc, W = G.S, G.nc, L.W
    with ExitStack() as st:
        cs = G.sb("cs", [128, 16], BF16, stack=st)
        S.op("act", lambda e: e.activation(out=cs[:, :], in_=V(L, "c", 0, 16), func=AF.Silu), reads=[L.vec], writes=[cs])
        wt = [G.sb("modwt%d" % i, [128, 16, 512], BF16, stack=st) for i in range(2)]
        pm = G.ps("pmod", [128, 96], stack=st)
        mw = W["mod_w"].rearrange("(k p) n -> p k n", p=128)
        for j4 in range(24):
            w = wt[j4 % 2]
            S.dma("pool", w[:, :, :], mw[:, :, j4 * 512:(j4 + 1) * 512], writes=[w])
            for jj in range(4):
                j = j4 * 4 + jj
                for k in range(16):
                    S.op("pe", lambda e, w=w, jj=jj, k=k, j=j: e.matmul(pm[:, j:j + 1], w[:, k, jj * 128:(jj + 1) * 128], cs[:, k:k + 1],
                                                                      start=(k == 0), stop=(k == 15)),
                         reads=[w, cs], writes=[pm])
        S.op("dve", lambda e: e.tensor_tensor(out=L.modv[:, :], in0=pm[:, :], in1=V(L, "mod_b", 0, 96), op=ALU.add),
             reads=[pm, L.vec], writes=[L.modv])
        mv, dr = L.modv, L.der
        S.op("dve", lambda e: e.scalar_tensor_tensor(out=dr[:, 0:16], in0=mv[:, 16:32], scalar=1.0, in1=V(L, "norm_mix", 0, 16),
                                                     op0=ALU.add, op1=ALU.mult), reads=[mv, L.vec], writes=[dr])
        S.op("dve", lambda e: e.tensor_copy(out=dr[:, 16:32], in_=mv[:, 0:16]), reads=[mv], writes=[dr])
        S.op("dve", lambda e: e.tensor_copy(out=dr[:, 32:48], in_=mv[:, 32:48]), reads=[mv], writes=[dr])
        S.op("dve", lambda e: e.scalar_tensor_tensor(out=dr[:, 48:64], in0=mv[:, 64:80], scalar=1.0, in1=V(L, "norm_ffn", 0, 16),
                                                     op0=ALU.add, op1=ALU.mult), reads=[mv, L.vec], writes=[dr])
        S.op("dve", lambda e: e.tensor_copy(out=dr[:, 64:80], in_=mv[:, 48:64]), reads=[mv], writes=[dr])
        S.op("dve", lambda e: e.tensor_copy(out=dr[:, 80:96], in_=mv[:, 80:96]), reads=[mv], writes=[dr])
        S.op("dve", lambda e: e.tensor_scalar(out=L.om[:, :], in0=L.vec[:, :], scalar1=-1.0, scalar2=1.0, op0=ALU.mult, op1=ALU.add),
             reads=[L.vec], writes=[L.om])
        if "mod" in G.dbg and L.l == 0:
            S.dma("sp", G.dbg["mod"], L.modv[:, :], reads=[L.modv])
        S.barrier()


def phase_norm(G, L, x_dram, ao, bo, router=None):
    S, nc = G.S, G.nc
    with ExitStack() as st:
        xt = [G.sb("nx%d" % i, [128, D], stack=st) for i in range(2)]
        sq = G.sb("nsq", [128, D], stack=st)
        xn = [G.sb("nxn%d" % i, [128, D], stack=st) for i in range(2)]
        ss = [G.sb("nss%d" % i, [128, 2], stack=st) for i in range(2)]
        hst = [G.sb("nhs%d" % i, [128, 16, 128], BF16, stack=st) for i in range(2)]
        pt = [G.ps("npt%d" % i, [128, 512], stack=st) for i in range(2)]
        hview = G.hT.rearrange("(k p) t -> p k t", p=128)
        import os
        RL = int(os.environ.get("ROUTER_LVL", 9))
        if router is not None:
            rsb = G.sb("rsb", [128, 16, NEXP], stack=st)
            S.dma("sp", rsb[:, :, :], router.rearrange("p (k e) -> p k e", k=16), writes=[rsb])
            hf = [G.sb("nhf%d" % i, [128, 16, 128], stack=st) for i in range(2)]
            plg = G.ps("nplg", [128, NEXP], stack=st)
            lg = G.sb("nlg", [128, NEXP], stack=st)
            t8 = G.sb("nt8", [128, 8], stack=st)
            sm = G.sb("nsm", [128, 8], stack=st)
            ge = G.sb("nge", [128, NEXP], stack=st)
            gsel = G.sb("ngsel", [128, NEXP], stack=st)
            pgt = G.ps("npgt", [NEXP, 128], stack=st)
            gts = G.sb("ngts", [NEXP, T], stack=st)
        for tt in range(NT):
            x = xt[tt % 2]
            n_ = xn[tt % 2]
            s_ = ss[tt % 2]
            S.dma("sp", x[:, :], x_dram[tt * 128:(tt + 1) * 128, :], writes=[x])
            S.op("act", lambda e, x=x: e.activation(out=sq[:, :], in_=x[:, :], func=AF.Square), reads=[x], writes=[sq])
            S.op("dve", lambda e, s_=s_: e.tensor_reduce(out=s_[:, 0:1], in_=sq[:, :], axis=AX.X, op=ALU.add), reads=[sq], writes=[s_])
            S.op("act", lambda e, s_=s_: e.activation(out=s_[:, 1:2], in_=s_[:, 0:1], func=AF.Sqrt, scale=1.0 / D, bias=G.cst.t[:, CO_EPS6:CO_EPS6 + 1]),
                 reads=[s_, G.cst], writes=[s_])
            S.op("dve", lambda e, s_=s_: e.reciprocal(out=s_[:, 0:1], in_=s_[:, 1:2]), reads=[s_], writes=[s_])
            S.op("act", lambda e, x=x, n_=n_, s_=s_: e.activation(out=n_[:, :], in_=x[:, :], func=AF.Copy, scale=s_[:, 0:1]),
                 reads=[x, s_], writes=[n_])
            hs = hst[tt % 2]
            for k4 in range(4):
                p_ = pt[k4 % 2]
                for kk in range(4):
                    k = k4 * 4 + kk
                    S.op("pe", lambda e, p_=p_, kk=kk, k=k, n_=n_: e.transpose(out=p_[:, kk * 128:(kk + 1) * 128], in_=n_[:, k * 128:(k + 1) * 128],
                                                                              identity=G.ident), reads=[n_, G.cst], writes=[p_])
                for kk in range(4):
                    k = k4 * 4 + kk
                    S.op("act", lambda e, p_=p_, kk=kk, k=k, hs=hs: e.activation(out=hs[:, k, :], in_=p_[:, kk * 128:(kk + 1) * 128], func=AF.Identity,
                                                                                scale=L.der[:, ao + k:ao + k + 1], bias=L.der[:, bo + k:bo + k + 1]),
                         reads=[p_, L.der], writes=[hs])
                    if router is not None and RL >= 2:
                        h_ = hf[tt % 2]
                        S.op("act", lambda e, p_=p_, kk=kk, k=k, h_=h_: e.activation(out=h_[:, k, :], in_=p_[:, kk * 128:(kk + 1) * 128], func=AF.Identity,
                                                                                    scale=L.der[:, ao + k:ao + k + 1], bias=L.der[:, bo + k:bo + k + 1]),
                             reads=[p_, L.der], writes=[h_])
            if router is not None and RL >= 2:
                h_ = hf[tt % 2]
                for k in range(16):
                    S.op("pe", lambda e, k=k, h_=h_: e.matmul(plg[:, :], h_[:, k, :], rsb[:, k, :], start=(k == 0), stop=(k == 15)),
                         reads=[h_, rsb], writes=[plg])
            S.dma("sp", hview[:, :, tt * 128:(tt + 1) * 128], hs[:, :, :], reads=[hs])
            if router is not None and RL >= 3:
                S.op("dve", lambda e: e.tensor_copy(out=lg[:, :], in_=plg[:, :]), reads=[plg], writes=[lg])
                S.op("dve", lambda e: e.max(out=t8[:, :], in_=lg[:, :]), reads=[lg], writes=[t8])
            if router is not None and RL >= 4:
                S.op("dve", lambda e: e.tensor_scalar(out=sm[:, 0:1], in0=t8[:, 0:1], scalar1=-1.0, scalar2=None, op0=ALU.mult), reads=[t8], writes=[sm])
                S.op("act", lambda e: e.activation(out=sm[:, 1:2], in_=t8[:, 1:2], func=AF.Exp, bias=sm[:, 0:1]), reads=[t8, sm], writes=[sm])
                S.op("dve", lambda e: e.tensor_scalar(out=sm[:, 3:4], in0=sm[:, 1:2], scalar1=1.0, scalar2=None, op0=ALU.add), reads=[sm], writes=[sm])
                S.op("dve", lambda e: e.reciprocal(out=sm[:, 2:3], in_=sm[:, 3:4]), reads=[sm], writes=[sm])
                S.op("act", lambda e: e.activation(out=ge[:, :], in_=lg[:, :], func=AF.Exp, bias=sm[:, 0:1]), reads=[lg, sm], writes=[ge])
                S.op("dve", lambda e: e.tensor_scalar(out=gsel[:, :], in0=lg[:, :], scalar1=t8[:, 1:2], scalar2=sm[:, 2:3], op0=ALU.is_ge, op1=ALU.mult),
                     reads=[lg, t8, sm], writes=[gsel])
                S.op("dve", lambda e: e.tensor_tensor(out=ge[:, :], in0=ge[:, :], in1=gsel[:, :], op=ALU.mult), reads=[ge, gsel], writes=[ge])
            if router is not None and RL >= 5:
                S.op("pe", lambda e: e.transpose(out=pgt[:, :], in_=ge[:, :], identity=G.ident), reads=[ge, G.cst], writes=[pgt])
                S.op("act", lambda e, tt=tt: e.copy(out=gts[:, tt * 128:(tt + 1) * 128], in_=pgt[:, :]), reads=[pgt], writes=[gts])
        if router is not None and RL >= 6:
            S.dma("sp", G.gT, gts[:, :], reads=[gts])
        S.barrier()


def proj_chunks(W):
    ch = []
    for i in range(8):
        ch.append((i * 128, 128, i * 128, "mu_r", i))
    for i in range(8):
        ch.append((1024 + i * 128, 128, 1024 + i * 128, "mu_k", i))
    for i in range(8):
        ch.append((2048 + i * 128, 128, 2048 + i * 128, "mu_v", i))
    ch.append((3072, 64, 3072, "mu_zw", 0))
    ch.append((3136, 64, 3136, "mu_za", 0))
    ch.append((3200, 128, 3200, "mu_zg", 0))
    ch.append((3328, 32, 3328, "mu_zg2", 0))
    nr = 3360
    if W["vr"]:
        ch.append((3360, 32, 3360, "mu_zv", 0))
        nr = 3392
    for i in range(24):
        ch.append((nr + i * 128, 128, 3392 + i * 128, None, 0))
    return ch


ZQ = 3392


def phase_proj(G, L):
    S, nc, W = G.S, G.nc, L.W
    with ExitStack() as st:
        hT = G.sb("pj_hT", [128, 16, T], BF16, stack=st)
        hb = [Buf() for _ in range(4)]
        hview = G.hT.rearrange("(k p) t -> p k t", p=128)
        for q in range(4):
            S.dma("sp", hT[:, :, q * 512:(q + 1) * 512], hview[:, :, q * 512:(q + 1) * 512], writes=[hb[q]])
        wt = [G.sb("pj_w%d" % i, [128, 16, 128], BF16, stack=st) for i in range(2)]
        stg = [G.sb("pj_s%d" % i, [128, T + 1], stack=st) for i in range(2)]
        tmp = G.sb("pj_tmp", [128, T], stack=st)
        zt = [G.sb("pj_z%d" % i, [128, T], stack=st) for i in range(2)]
        pp = [G.ps("pj_p%d" % i, [128, 512], stack=st) for i in range(2)]
        for s_ in stg:
            S.op("dve", lambda e, s_=s_: e.memset(s_[:, 0:1], 0.0), writes=[s_])
        wv = W["w_in"].rearrange("(k p) n -> p k n", p=128)
        for ci, (c0, n, r0, mu, mj) in enumerate(proj_chunks(W)):
            w = wt[ci % 2]
            sg = stg[ci % 2]
            S.dma("pool", w[:, :, 0:n], wv[:, :, c0:c0 + n], writes=[w])
            for q in range(4):
                p_ = pp[q % 2]
                for k in range(16):
                    S.op("pe", lambda e, p_=p_, w=w, k=k, q=q, n=n: e.matmul(p_[0:n, :], w[:, k, 0:n], hT[:, k, q * 512:(q + 1) * 512],
                                                                            start=(k == 0), stop=(k == 15)), reads=[w, hb[q]], writes=[p_])
                S.op("act", lambda e, p_=p_, sg=sg, q=q, n=n: e.copy(out=sg[0:n, 1 + q * 512:1 + (q + 1) * 512], in_=p_[0:n, :]), reads=[p_], writes=[sg])
            if mu is not None:
                z = zt[ci % 2]
                S.op("dve", lambda e, sg=sg, n=n, mu=mu, mj=mj: e.tensor_scalar(out=tmp[0:n, :], in0=sg[0:n, 1:T + 1], scalar1=L.om[0:n, VO[mu] + mj:VO[mu] + mj + 1],
                                                                               scalar2=None, op0=ALU.mult), reads=[sg, L.om], writes=[tmp])
                S.op("dve", lambda e, sg=sg, n=n, mu=mu, mj=mj, z=z: e.scalar_tensor_tensor(out=z[0:n, :], in0=sg[0:n, 0:T], scalar=V(L, mu, mj, 1, 0, n), in1=tmp[0:n, :],
                                                                                          op0=ALU.mult, op1=ALU.add), reads=[sg, L.vec, tmp], writes=[z])
                S.dma("sp", G.zf[r0:r0 + n, :], z[0:n, :], reads=[z])
            else:
                S.dma("sp", G.zf[r0:r0 + n, :], sg[0:n, 1:T + 1], reads=[sg])
        S.barrier()


def phase_rwkv_pre(G, L):
    S, nc, W = G.S, G.nc, L.W
    vr = W["vr"]
    with ExitStack() as st:
        tzw = G.sb("rp_tzw", [64, T], stack=st)
        za = G.sb("rp_za", [64, T], stack=st)
        sga = G.sb("rp_sga", [128, T], stack=st)
        sgb = G.sb("rp_sgb", [32, T], stack=st)
        S.dma("sp", tzw[:, :], G.zf[3072:3136, :], writes=[tzw])
        S.dma("sp", za[:, :], G.zf[3136:3200, :], writes=[za])
        S.dma("sp", sga[:, :], G.zf[3200:3328, :], writes=[sga])
        S.dma("sp", sgb[:, :], G.zf[3328:3360, :], writes=[sgb])
        S.op("act", lambda e: e.activation(out=tzw[:, :], in_=tzw[:, :], func=AF.Tanh), reads=[tzw], writes=[tzw])
        S.op("act", lambda e: e.activation(out=sga[:, :], in_=sga[:, :], func=AF.Sigmoid), reads=[sga], writes=[sga])
        S.op("act", lambda e: e.activation(out=sgb[:, :], in_=sgb[:, :], func=AF.Sigmoid), reads=[sgb], writes=[sgb])
        w2 = G.sb("rp_w2", [64, C], stack=st)
        a2 = G.sb("rp_a2", [64, C], stack=st)
        g2a = G.sb("rp_g2a", [128, C], stack=st)
        g2b = G.sb("rp_g2b", [32, C], stack=st)
        S.dma("sp", w2[:, :], W["w2"], writes=[w2])
        S.dma("sp", a2[:, :], W["a2"], writes=[a2])
        S.dma("sp", g2a[:, :], W["g2"][0:128, :], writes=[g2a])
        S.dma("sp", g2b[:, :], W["g2"][128:160, :], writes=[g2b])
        if vr:
            zv = G.sb("rp_zv", [32, T], stack=st)
            v2 = G.sb("rp_v2", [32, C], stack=st)
            S.dma("sp", zv[:, :], G.zf[3360:3392, :], writes=[zv])
            S.dma("sp", v2[:, :], W["v2"], writes=[v2])

        def mk(name, n=2, dt=F32, shape=(128, 512)):
            return [G.sb("rp_%s%d" % (name, i), list(shape), dt, stack=st) for i in range(n)]

        rt, kt, vt = mk("r"), mk("k"), mk("v")
        vft = mk("vf")
        dec, alr, gg = mk("dec"), mk("alr"), mk("g")
        t1, t2, t3 = mk("t1"), mk("t2"), mk("t3")
        arec, brec, kmod, bon = mk("arec"), mk("brec"), mk("kmod"), mk("bon")
        vtb = mk("vtb", 2, BF16, (128, 4, 128))
        pA = [G.ps("rp_pA%d" % i, [128, 512], stack=st) for i in range(2)]
        pB = [G.ps("rp_pB%d" % i, [128, 512], stack=st) for i in range(2)]
        pC = [G.ps("rp_pC%d" % i, [128, 512], stack=st) for i in range(2)]
        vtmv = G.vtm.rearrange("(s p) c -> p s c", p=128)
        it = 0
        for c in range(8):
            cs_ = slice(c * 128, (c + 1) * 128)
            for q in range(4):
                i = it % 2
                it += 1
                ts_ = slice(q * 512, (q + 1) * 512)
                r_, k_, v_ = rt[i], kt[i], vt[i]
                S.dma("sp", r_[:, :], G.zf[c * 128:(c + 1) * 128, ts_], writes=[r_])
                S.dma("sp", k_[:, :], G.zf[1024 + c * 128:1024 + (c + 1) * 128, ts_], writes=[k_])
                S.dma("sp", v_[:, :], G.zf[2048 + c * 128:2048 + (c + 1) * 128, ts_], writes=[v_])
                pa, pb, pc = pA[i], pB[i], pC[i]
                S.op("pe", lambda e, pa=pa: e.matmul(pa[:, :], w2[:, cs_], tzw[:, ts_], start=True, stop=True), reads=[w2, tzw], writes=[pa])
                d_ = dec[i]
                S.op("act", lambda e, pa=pa, d_=d_: e.activation(out=d_[:, :], in_=pa[:, :], func=AF.Sigmoid, bias=V(L, "w0", c)), reads=[pa, L.vec], writes=[d_])
                S.op("act", lambda e, d_=d_: e.activation(out=d_[:, :], in_=d_[:, :], func=AF.Exp, scale=-0.6065306597126334), reads=[d_], writes=[d_])
                S.dma("sp", G.rw["w"][cs_, ts_], d_[:, :], reads=[d_])
                S.op("pe", lambda e, pb=pb: e.matmul(pb[:, :], a2[:, cs_], za[:, ts_], start=True, stop=True), reads=[a2, za], writes=[pb])
                al = alr[i]
                S.op("act", lambda e, pb=pb, al=al: e.activation(out=al[:, :], in_=pb[:, :], func=AF.Sigmoid, bias=V(L, "a0", c)), reads=[pb, L.vec], writes=[al])
                S.op("pe", lambda e, pc=pc: e.matmul(pc[:, :], g2a[:, cs_], sga[:, ts_], start=True, stop=False), reads=[g2a, sga], writes=[pc])
                S.op("pe", lambda e, pc=pc: e.matmul(pc[:, :], g2b[:, cs_], sgb[:, ts_], start=False, stop=True), reads=[g2b, sgb], writes=[pc])
                g_ = gg[i]
                S.op("act", lambda e, pc=pc, g_=g_: e.copy(out=g_[:, :], in_=pc[:, :]), reads=[pc], writes=[g_])
                S.dma("sp", G.rw["g"][cs_, ts_], g_[:, :], reads=[g_])
                if vr:
                    vf = vft[i]
                    S.dma("sp", vf[:, :], G.vfirst[cs_, ts_], writes=[vf])
                    S.op("pe", lambda e, pa=pa: e.matmul(pa[:, :], v2[:, cs_], zv[:, ts_], start=True, stop=True), reads=[v2, zv], writes=[pa])
                    sv = t1[i]
                    S.op("act", lambda e, pa=pa, sv=sv: e.activation(out=sv[:, :], in_=pa[:, :], func=AF.Sigmoid, bias=V(L, "v0", c)), reads=[pa, L.vec], writes=[sv])
                    S.op("dve", lambda e, vf=vf, v_=v_: e.tensor_tensor(out=vf[:, :], in0=vf[:, :], in1=v_[:, :], op=ALU.subtract), reads=[vf, v_], writes=[vf])
                    S.op("dve", lambda e, vf=vf, sv=sv: e.tensor_tensor(out=vf[:, :], in0=vf[:, :], in1=sv[:, :], op=ALU.mult), reads=[vf, sv], writes=[vf])
                    S.op("dve", lambda e, vf=vf, v_=v_: e.tensor_tensor(out=v_[:, :], in0=v_[:, :], in1=vf[:, :], op=ALU.add), reads=[vf, v_], writes=[v_])
                else:
                    S.dma("sp", G.vfirst[cs_, ts_], v_[:, :], reads=[v_])
                S.dma("sp", G.rw["v"][cs_, ts_], v_[:, :], reads=[v_])
                kk = t2[i]
                sq = t3[i]
                S.op("dve", lambda e, kk=kk, k_=k_: e.tensor_scalar(out=kk[:, :], in0=k_[:, :], scalar1=V(L, "k_k", c), scalar2=None, op0=ALU.mult),
                     reads=[k_, L.vec], writes=[kk])
                S.op("act", lambda e, kk=kk, sq=sq: e.activation(out=sq[:, :], in_=kk[:, :], func=AF.Square), reads=[kk], writes=[sq])
                S.op("pe", lambda e, pb=pb, sq=sq: e.matmul(pb[:, :], G.bones, sq[:, :], start=True, stop=True), reads=[sq, G.cst], writes=[pb])
                S.op("dve", lambda e, pb=pb, sq=sq: e.tensor_scalar(out=sq[:, :], in0=pb[:, :], scalar1=1e-24, scalar2=None, op0=ALU.max),
                     reads=[pb], writes=[sq])
                S.op("act", lambda e, sq=sq: e.activation(out=sq[:, :], in_=sq[:, :], func=AF.Sqrt), reads=[sq], writes=[sq])
                S.op("dve", lambda e, sq=sq: e.reciprocal(out=sq[:, :], in_=sq[:, :]), reads=[sq], writes=[sq])
                S.op("dve", lambda e, kk=kk, sq=sq: e.tensor_tensor(out=kk[:, :], in0=kk[:, :], in1=sq[:, :], op=ALU.mult), reads=[kk, sq], writes=[kk])
                ar, br = arec[i], brec[i]
                S.op("act", lambda e, ar=ar, kk=kk: e.mul(out=ar[:, :], in_=kk[:, :], mul=-1.0), reads=[kk], writes=[ar])
                S.dma("sp", G.rw["a"][cs_, ts_], ar[:, :], reads=[ar])
                S.op("dve", lambda e, br=br, kk=kk, al=al: e.tensor_tensor(out=br[:, :], in0=kk[:, :], in1=al[:, :], op=ALU.mult), reads=[kk, al], writes=[br])
                S.dma("sp", G.rw["b"][cs_, ts_], br[:, :], reads=[br])
                km = kmod[i]
                S.op("dve", lambda e, al=al, km=km: e.tensor_scalar(out=km[:, :], in0=al[:, :], scalar1=-1.0, scalar2=V(L, "k_a", c), op0=ALU.add, op1=ALU.mult),
                     reads=[al, L.vec], writes=[km])
                S.op("dve", lambda e, km=km, k_=k_: e.scalar_tensor_tensor(out=km[:, :], in0=km[:, :], scalar=1.0, in1=k_[:, :], op0=ALU.add, op1=ALU.mult),
                     reads=[km, k_], writes=[km])
                S.dma("sp", G.rw["kmod"][cs_, ts_], km[:, :], reads=[km])
                bo_ = bon[i]
                S.op("dve", lambda e, bo_=bo_, r_=r_, km=km: e.scalar_tensor_tensor(out=bo_[:, :], in0=r_[:, :], scalar=V(L, "r_k", c), in1=km[:, :],
                                                                                   op0=ALU.mult, op1=ALU.mult), reads=[r_, km, L.vec], writes=[bo_])
                S.op("pe", lambda e, pc=pc, bo_=bo_: e.matmul(pc[:, :], G.bones, bo_[:, :], start=True, stop=True), reads=[bo_, G.cst], writes=[pc])
                S.op("dve", lambda e, pc=pc, bo_=bo_, v_=v_: e.tensor_tensor(out=bo_[:, :], in0=pc[:, :], in1=v_[:, :], op=ALU.mult), reads=[pc, v_], writes=[bo_])
                S.dma("sp", G.rw["bonus"][cs_, ts_], bo_[:, :], reads=[bo_])
                vb = vtb[i]
                for s4 in range(4):
                    S.op("pe", lambda e, pa=pa, v_=v_, s4=s4: e.transpose(out=pa[:, s4 * 128:(s4 + 1) * 128], in_=v_[:, s4 * 128:(s4 + 1) * 128], identity=G.ident),
                         reads=[v_, G.cst], writes=[pa])
                S.op("act", lambda e, pa=pa, vb=vb: e.copy(out=vb[:, :, :], in_=pa[:, :].rearrange("p (s c) -> p s c", s=4)), reads=[pa], writes=[vb])
                S.dma("sp", vtmv[:, q * 4:(q + 1) * 4, cs_], vb[:, :, :], reads=[vb])
        S.barrier()


def phase_rec(G, L):
    import os
    S, nc = G.S, G.nc
    TC = 128
    NS = 4
    with ExitStack() as st:
        St = G.sb("rc_S", [128, 2, 8, 64], stack=st)
        Sb = [[Buf() for _ in range(8)] for _ in range(2)]
        S.op("dve", lambda e: e.memset(St[:, :, :, :], 0.0), writes=[b for r in Sb for b in r])
        names = ("w", "b", "a", "r", "kmod")
        inb = {n: [G.sb("rc_%s%d" % (n, i), [128, 8, TC], stack=st) for i in range(2)] for n in names}
        vtb = [G.sb("rc_v%d" % i, [128, C], BF16, stack=st) for i in range(2)]
        tmpk = [G.sb("rc_tmp%d" % i, [128, 8, 64], stack=st) for i in range(2)]
        tmpb = [[Buf() for _ in range(8)] for _ in range(2)]
        psa = [G.ps("rc_psa%d" % i, [128, 2, 64], stack=st) for i in range(NS)]
        psv = [G.ps("rc_psv%d" % i, [128, 8, 64], stack=st) for i in range(2)]
        psy = [G.ps("rc_psy%d" % i, [128, 8, 64], stack=st) for i in range(2)]
        ysb = [G.sb("rc_y%d" % i, [128, 8, TC], stack=st) for i in range(2)]
        yview = G.rw["y"].rearrange("(g p) t -> p g t", p=128)
        for ch in range(int(os.environ.get("REC_CHUNKS", T // TC))):
            t0 = ch * TC
            ib = {n: inb[n][ch % 2] for n in names}
            vb = vtb[ch % 2]
            for n in names:
                src = G.zf[0:C, :] if n == "r" else G.rw[n]
                S.dma("sp", ib[n][:, :, :], src.rearrange("(g p) t -> p g t", p=128)[:, :, t0:t0 + TC], writes=[ib[n]])
            S.dma("sp", vb[:, :], G.vtm[t0:t0 + TC, :], writes=[vb])
            ys = ysb[ch % 2]

            def emit_kv(tl_):
                par = (t0 + tl_) % 2
                pv = psv[par]
                for g in range(8):
                    for h2 in range(2):
                        p0, p1 = h2 * 64, h2 * 64 + 64
                        kw = {"tile_position": (0, 64)} if h2 else {}
                        hc = (g * 2 + h2) * 64
                        S.op("pe", lambda e, g=g, p0=p0, p1=p1, kw=kw, hc=hc: e.matmul(
                            pv[p0:p1, g, :], G.identb[:, tl_:tl_ + 1].to_broadcast([128, 64]), vb[:, hc:hc + 64], start=True, stop=True, **kw),
                            reads=[vb, G.cstb], writes=[pv])
                for g in range(8):
                    S.op("act", lambda e, g=g: e.activation(out=tmpk[par][:, g, :], in_=pv[:, g, :], func=AF.Copy, scale=ib["kmod"][:, g, tl_:tl_ + 1]),
                         reads=[pv, ib["kmod"]], writes=[tmpb[par][g]])

            def emit_mm3(pend, s_):
                tl_, n__, py_, yc_ = pend
                for gi in range(2):
                    g = s_ * 2 + gi
                    for h2 in range(2):
                        p0, p1 = h2 * 64, h2 * 64 + 64
                        kw = {"tile_position": (64, 64)} if h2 else {}
                        S.op("pe", lambda e, g=g, p0=p0, p1=p1, kw=kw: e.matmul(
                            py_[p0:p1, g, yc_:yc_ + 1], St[p0:p1, n__, g, :], ib["r"][p0:p1, g, tl_:tl_ + 1], start=True, stop=True, **kw),
                            reads=[Sb[n__][g], ib["r"]], writes=[py_])

            def emit_ycopy(pend):
                tl_, n__, py_, yc_ = pend
                if yc_ == 63:
                    half = tl_ // 64
                    S.op("act", lambda e: e.copy(out=ys[:, :, half * 64:(half + 1) * 64], in_=py_[:, :, :]), reads=[py_], writes=[ys])

            emit_kv(0)
            pend = None
            for tl in range(TC):
                t = t0 + tl
                o, n_ = t % 2, (t + 1) % 2
                par = t % 2
                py = psy[(tl // 64) % 2]
                yc = tl % 64
                for s_ in range(NS):
                    pa_ = psa[s_]
                    for gi in range(2):
                        g = s_ * 2 + gi
                        for h2 in range(2):
                            p0, p1 = h2 * 64, h2 * 64 + 64
                            kw = {"tile_position": (64, 64)} if h2 else {}
                            S.op("pe", lambda e, g=g, gi=gi, pa_=pa_, p0=p0, p1=p1, kw=kw: e.matmul(
                                pa_[p0:p1, gi, :], ib["a"][p0:p1, g, tl:tl + 1].to_broadcast([64, 64]), St[p0:p1, o, g, :], start=True, stop=True, **kw),
                                reads=[Sb[o][g], ib["a"]], writes=[pa_])
                    if pend is not None:
                        emit_mm3(pend, s_)
                if pend is not None:
                    emit_ycopy(pend)
                if tl + 1 < TC:
                    emit_kv(tl + 1)

                def op1(s_):
                    g0 = s_ * 2
                    S.op("pool", lambda e: e.tensor_tensor(
                        out=St[:, n_, g0:g0 + 2, :], in0=St[:, o, g0:g0 + 2, :], in1=ib["w"][:, g0:g0 + 2, tl:tl + 1].to_broadcast([128, 2, 64]), op=ALU.mult),
                        reads=[Sb[o][g0], Sb[o][g0 + 1], ib["w"]], writes=[Sb[n_][g0], Sb[n_][g0 + 1]])
                    S.op("pool", lambda e: e.tensor_tensor(
                        out=St[:, n_, g0:g0 + 2, :], in0=St[:, n_, g0:g0 + 2, :], in1=tmpk[par][:, g0:g0 + 2, :], op=ALU.add),
                        reads=[tmpb[par][g0], tmpb[par][g0 + 1], Sb[n_][g0], Sb[n_][g0 + 1]], writes=[Sb[n_][g0], Sb[n_][g0 + 1]])

                def op2(s_):
                    pa_ = psa[s_]
                    for gi in range(2):
                        g = s_ * 2 + gi
                        S.op("dve", lambda e, g=g, gi=gi: e.scalar_tensor_tensor(
                            out=St[:, n_, g, :], in0=pa_[:, gi, :], scalar=ib["b"][:, g, tl:tl + 1], in1=St[:, n_, g, :], op0=ALU.mult, op1=ALU.add),
                            reads=[pa_, ib["b"], Sb[n_][g]], writes=[Sb[n_][g]])

                for s_ in range(NS):
                    op1(s_)
                for s_ in range(NS):
                    op2(s_)
                pend = (tl, n_, py, yc)
            for s_ in range(NS):
                emit_mm3(pend, s_)
            emit_ycopy(pend)
            S.dma("sp", yview[:, :, t0:t0 + TC], ys[:, :, :], reads=[ys])
        S.barrier()


def phase_rwkv_post(G, L):
    S, nc = G.S, G.nc
    with ExitStack() as st:
        def mk(name, n=2, dt=F32, shape=(128, 512)):
            return [G.sb("rq_%s%d" % (name, i), list(shape), dt, stack=st) for i in range(n)]
        yt, bt, gt, dt_, sq, ob = mk("y"), mk("b"), mk("g"), mk("d"), mk("sq"), mk("ob", 2, BF16)
        pA = [G.ps("rq_pA%d" % i, [128, 512], stack=st) for i in range(2)]
        pB = [G.ps("rq_pB%d" % i, [128, 512], stack=st) for i in range(2)]
        it = 0
        for c in range(8):
            cs_ = slice(c * 128, (c + 1) * 128)
            for q in range(4):
                i = it % 2
                it += 1
                ts_ = slice(q * 512, (q + 1) * 512)
                y_, b_, g_, d_, s_, o_ = yt[i], bt[i], gt[i], dt_[i], sq[i], ob[i]
                pa, pb = pA[i], pB[i]
                S.dma("sp", y_[:, :], G.rw["y"][cs_, ts_], writes=[y_])
                S.dma("sp", b_[:, :], G.rw["bonus"][cs_, ts_], writes=[b_])
                S.dma("sp", g_[:, :], G.rw["g"][cs_, ts_], writes=[g_])
                S.op("pe", lambda e, pa=pa, y_=y_: e.matmul(pa[:, :], G.bones, y_[:, :], start=True, stop=True), reads=[y_, G.cst], writes=[pa])
                S.op("dve", lambda e, pa=pa, y_=y_, d_=d_: e.scalar_tensor_tensor(out=d_[:, :], in0=pa[:, :], scalar=-1.0 / 64, in1=y_[:, :], op0=ALU.mult, op1=ALU.add),
                     reads=[pa, y_], writes=[d_])
                S.op("act", lambda e, d_=d_, s_=s_: e.activation(out=s_[:, :], in_=d_[:, :], func=AF.Square), reads=[d_], writes=[s_])
                S.op("pe", lambda e, pb=pb, s_=s_: e.matmul(pb[:, :], G.bones, s_[:, :], start=True, stop=True), reads=[s_, G.cst], writes=[pb])
                S.op("act", lambda e, pb=pb, s_=s_: e.activation(out=s_[:, :], in_=pb[:, :], func=AF.Sqrt, scale=1.0 / 64, bias=G.cst.t[:, CO_EPSGN:CO_EPSGN + 1]),
                     reads=[pb, G.cst], writes=[s_])
                S.op("dve", lambda e, s_=s_: e.reciprocal(out=s_[:, :], in_=s_[:, :]), reads=[s_], writes=[s_])
                S.op("dve", lambda e, s_=s_, d_=d_: e.tensor_tensor(out=d_[:, :], in0=d_[:, :], in1=s_[:, :], op=ALU.mult), reads=[s_, d_], writes=[d_])
                S.op("dve", lambda e, d_=d_: e.tensor_scalar(out=d_[:, :], in0=d_[:, :], scalar1=V(L, "ln_w", c), scalar2=V(L, "ln_b", c), op0=ALU.mult, op1=ALU.add),
                     reads=[d_, L.vec], writes=[d_])
                S.op("dve", lambda e, d_=d_, b_=b_: e.tensor_tensor(out=d_[:, :], in0=d_[:, :], in1=b_[:, :], op=ALU.add), reads=[d_, b_], writes=[d_])
                S.op("dve", lambda e, d_=d_, g_=g_, o_=o_: e.tensor_tensor(out=o_[:, :], in0=d_[:, :], in1=g_[:, :], op=ALU.mult), reads=[d_, g_], writes=[o_])
                S.dma("sp", G.mixT[cs_, ts_], o_[:, :], reads=[o_])
        S.barrier()


def phase_moba(G, L):
    S, nc = G.S, G.nc
    l = L.l
    with ExitStack() as st:
        qf = [G.sb("mb_qf%d" % i, [128, T], stack=st) for i in range(2)]
        kf = [G.sb("mb_kf%d" % i, [128, T], stack=st) for i in range(2)]
        vf = [G.sb("mb_vf%d" % i, [128, T], stack=st) for i in range(2)]
        qb = G.sb("mb_qb", [128, T], BF16, stack=st)
        kb = G.sb("mb_kb", [128, T], BF16, stack=st)
        sqt = G.sb("mb_sq", [128, T], stack=st)
        km = G.sb("mb_km", [128, NB], stack=st)
        RBf = G.sb("mb_RBf", [33, T], stack=st)
        RB = G.sb("mb_RB", [33, T], BF16, stack=st)
        rowt = G.sb("mb_row", [33, T], stack=st)
        kx = G.sb("mb_kx", [33, 2], stack=st)
        va = G.sb("mb_va", [128, 16, 132], BF16, stack=st)
        gm = G.sb("mb_gm", [128, 8], stack=st)
        t8 = G.sb("mb_t8", [128, 8], stack=st)
        Et = [G.sb("mb_E%d" % i, [128, 256], BF16, stack=st) for i in range(2)]
        ot = G.sb("mb_o", [128, 128], stack=st)
        osq = G.sb("mb_osq", [128, 128], stack=st)
        osm = G.sb("mb_osm", [128, 4], stack=st)
        ymT = G.sb("mb_ymT", [128, T], BF16, stack=st)
        gain = G.sb("mb_gain", [128, 1024], stack=st)
        S.dma("sp", gain[:, :], G.rowp[:, RO_GAIN[l]:RO_GAIN[l] + 1024], writes=[gain])
        pq = G.ps("mb_pq", [33, 512], stack=st)
        pg = G.ps("mb_pg", [128, 8], stack=st)
        pt8 = G.ps("mb_pt8", [8, 128], stack=st)
        ptr = G.ps("mb_ptr", [128, 128], stack=st)
        pss = [G.ps("mb_ps%d" % i, [128, 256], stack=st) for i in range(2)]
        pso = [G.ps("mb_po%d" % i, [128, 132], stack=st) for i in range(2)]
        S.op("dve", lambda e: e.memset(RBf[:, :], 0.0), writes=[RBf])
        S.op("dve", lambda e: e.memset(va[:, :, 128:129], 1.0), writes=[va])
        one32 = G.cst.t[:, CO_ONE32:CO_ONE32 + 33]
        for h in range(8):
            q_, k_, v_ = qf[h % 2], kf[h % 2], vf[h % 2]
            S.dma("sp", q_[:, :], G.zf[ZQ + h * 128:ZQ + (h + 1) * 128, :], writes=[q_])
            S.dma("sp", k_[:, :], G.zf[ZQ + 1024 + h * 128:ZQ + 1024 + (h + 1) * 128, :], writes=[k_])
            S.dma("sp", v_[:, :], G.zf[ZQ + 2048 + h * 128:ZQ + 2048 + (h + 1) * 128, :], writes=[v_])
            S.op("act", lambda e, q_=q_: e.copy(out=qb[:, :], in_=q_[:, :]), reads=[q_], writes=[qb])
            S.op("act", lambda e, k_=k_: e.copy(out=kb[:, :], in_=k_[:, :]), reads=[k_], writes=[kb])
            S.op("dve", lambda e, k_=k_: e.tensor_reduce(out=km[:, :], in_=k_[:, :].rearrange("p (n s) -> p n s", n=NB), axis=AX.X, op=ALU.add),
                 reads=[k_], writes=[km])
            S.op("dve", lambda e: e.tensor_scalar(out=km[:, :], in0=km[:, :], scalar1=1.0 / 256, scalar2=None, op0=ALU.mult), reads=[km], writes=[km])
            S.op("act", lambda e, k_=k_: e.activation(out=sqt[:, :], in_=k_[:, :], func=AF.Square), reads=[k_], writes=[sqt])
            for q4 in range(4):
                S.op("pe", lambda e, q4=q4: e.matmul(pq[:, :], one32, sqt[:, q4 * 512:(q4 + 1) * 512], start=True, stop=True), reads=[sqt, G.cst], writes=[pq])
                S.op("dve", lambda e, q4=q4: e.tensor_copy(out=rowt[32:33, q4 * 512:(q4 + 1) * 512], in_=pq[32:33, :]), reads=[pq], writes=[rowt])
            S.op("dve", lambda e: e.tensor_reduce(out=kx[32:33, 0:1], in_=rowt[32:33, :], axis=AX.X, op=ALU.max), reads=[rowt], writes=[kx])
            S.op("act", lambda e, q_=q_: e.activation(out=sqt[:, :], in_=q_[:, :], func=AF.Square), reads=[q_], writes=[sqt])
            coef = -SLOPES[h] / SCALE
            for q4 in range(4):
                S.op("pe", lambda e, q4=q4: e.matmul(pq[:, :], one32, sqt[:, q4 * 512:(q4 + 1) * 512], start=True, stop=True), reads=[sqt, G.cst], writes=[pq])
                S.op("act", lambda e, q4=q4: e.activation(out=rowt[32:33, q4 * 512:(q4 + 1) * 512], in_=pq[32:33, :], func=AF.Sqrt, scale=kx[32:33, 0:1]),
                     reads=[pq, kx], writes=[rowt])
            S.op("dve", lambda e: e.scalar_tensor_tensor(out=RBf[32:33, :], in0=G.cst.t[32:33, CO_TROW:CO_TROW + T], scalar=coef, in1=rowt[32:33, :],
                                                         op0=ALU.mult, op1=ALU.subtract), reads=[rowt, G.cst], writes=[RBf])
            for tt in range(NT):
                own = tt // 2
                S.op("pe", lambda e, tt=tt, q_=q_: e.matmul(pg[:, :], q_[:, tt * 128:(tt + 1) * 128], km[:, :], start=True, stop=True), reads=[q_, km], writes=[pg])
                S.op("dve", lambda e, own=own: e.tensor_tensor(out=gm[:, :], in0=pg[:, :], in1=G.cst.t[:, CO_PAST + own * 8:CO_PAST + own * 8 + 8], op=ALU.add),
                     reads=[pg, G.cst], writes=[gm])
                S.op("dve", lambda e: e.max(out=t8[:, :], in_=gm[:, :]), reads=[gm], writes=[t8])
                S.op("dve", lambda e: e.tensor_scalar(out=gm[:, :], in0=gm[:, :], scalar1=t8[:, 2:3], scalar2=None, op0=ALU.is_ge), reads=[gm, t8], writes=[gm])
                S.op("dve", lambda e: e.tensor_scalar(out=gm[:, :], in0=gm[:, :], scalar1=-1.0, scalar2=BIGR, op0=ALU.add, op1=ALU.mult), reads=[gm], writes=[gm])
                S.op("pe", lambda e: e.transpose(out=pt8[:, :], in_=gm[:, :], identity=G.ident), reads=[gm, G.cst], writes=[pt8])
                S.op("act", lambda e, tt=tt: e.copy(out=RBf[0:8, tt * 128:(tt + 1) * 128], in_=pt8[:, :]), reads=[pt8], writes=[RBf])
            S.op("act", lambda e: e.copy(out=RB[:, :], in_=RBf[:, :]), reads=[RBf], writes=[RB])
            for stl in range(NT):
                S.op("pe", lambda e, stl=stl, v_=v_: e.transpose(out=ptr[:, :], in_=v_[:, stl * 128:(stl + 1) * 128], identity=G.ident), reads=[v_, G.cst], writes=[ptr])
                S.op("act", lambda e, stl=stl: e.copy(out=va[:, stl, 0:128], in_=ptr[:, :]), reads=[ptr], writes=[va])
            ei = 0
            for qblk in range(NB):
                tq = slice(qblk * 256, (qblk + 1) * 256)
                for n in range(qblk + 1):
                    for kt in range(2):
                        stl = 2 * n + kt
                        p_ = pss[ei % 2]
                        E_ = Et[ei % 2]
                        ei += 1
                        own = (n == qblk)
                        S.op("pe", lambda e, p_=p_, stl=stl, tq=tq: e.matmul(p_[:, :], kb[:, stl * 128:(stl + 1) * 128], qb[:, tq], start=True, stop=False),
                             reads=[kb, qb], writes=[p_])
                        sc = CO_SELC + (8 if own else n)
                        S.op("pe", lambda e, p_=p_, sc=sc, tq=tq, own=own: e.matmul(p_[:, :], G.cstb.t[0:33, sc:sc + 1].to_broadcast([33, 128]), RB[:, tq],
                                                                                  start=False, stop=(not own)), reads=[RB, G.cstb], writes=[p_])
                        if own:
                            cm = CO_CM1 if kt else CO_CM0
                            S.op("pe", lambda e, p_=p_, cm=cm: e.matmul(p_[:, :], G.identb, G.cstb.t[:, cm:cm + 256], start=False, stop=True),
                                 reads=[G.cstb], writes=[p_])
                        cb = CO_COLB + h * 16 + stl
                        S.op("act", lambda e, p_=p_, E_=E_, cb=cb: e.activation(out=E_[:, :], in_=p_[:, :], func=AF.Exp, scale=SCALE, bias=G.cst.t[:, cb:cb + 1]),
                             reads=[p_, G.cst], writes=[E_])
                        for tsub in range(2):
                            if own and kt == 1 and tsub == 0:
                                continue
                            first = (n == 0 and kt == 0)
                            last = own and (kt == tsub)
                            S.op("pe", lambda e, tsub=tsub, E_=E_, stl=stl, first=first, last=last: e.matmul(
                                pso[tsub][:, 0:129], E_[:, tsub * 128:(tsub + 1) * 128], va[:, stl, 0:129], start=first, stop=last),
                                reads=[E_, va], writes=[pso[tsub]])
                for tsub in range(2):
                    po = pso[tsub]
                    tcol = qblk * 256 + tsub * 128
                    S.op("dve", lambda e, po=po: e.reciprocal(out=osm[:, 0:1], in_=po[:, 128:129]), reads=[po], writes=[osm])
                    S.op("act", lambda e, po=po: e.activation(out=ot[:, :], in_=po[:, 0:128], func=AF.Copy, scale=osm[:, 0:1]), reads=[po, osm], writes=[ot])
                    S.op("act", lambda e: e.activation(out=osq[:, :], in_=ot[:, :], func=AF.Square), reads=[ot], writes=[osq])
                    S.op("dve", lambda e: e.tensor_reduce(out=osm[:, 1:2], in_=osq[:, :], axis=AX.X, op=ALU.add), reads=[osq], writes=[osm])
                    S.op("act", lambda e: e.activation(out=osm[:, 2:3], in_=osm[:, 1:2], func=AF.Sqrt, scale=1.0 / 128, bias=G.cst.t[:, CO_EPS6:CO_EPS6 + 1]),
                         reads=[osm, G.cst], writes=[osm])
                    S.op("dve", lambda e: e.reciprocal(out=osm[:, 3:4], in_=osm[:, 2:3]), reads=[osm], writes=[osm])
                    S.op("dve", lambda e: e.scalar_tensor_tensor(out=ot[:, :], in0=ot[:, :], scalar=osm[:, 3:4], in1=gain[:, h * 128:(h + 1) * 128],
                                                                 op0=ALU.mult, op1=ALU.mult), reads=[ot, osm, gain], writes=[ot])
                    S.op("pe", lambda e: e.transpose(out=ptr[:, :], in_=ot[:, :], identity=G.ident), reads=[ot, G.cst], writes=[ptr])
                    S.op("act", lambda e, tcol=tcol: e.copy(out=ymT[:, tcol:tcol + 128], in_=ptr[:, :]), reads=[ptr], writes=[ymT])
            S.dma("sp", G.mixT[C + h * 128:C + (h + 1) * 128, :], ymT[:, :], reads=[ymT])
        S.barrier()


def bcast_rows(G, st, src16, name):
    S = G.S
    o = G.sb(name, [128, D], stack=st)
    tr = G.sb(name + "_tr", [16, 128], stack=st)
    p1 = G.ps(name + "_p1", [16, 128], stack=st)
    p2 = G.ps(name + "_p2", [128, 512], stack=st)
    src, srcb = src16
    S.op("pe", lambda e: e.transpose(out=p1[:, :], in_=src, identity=G.ident), reads=[srcb, G.cst], writes=[p1])
    S.op("act", lambda e: e.copy(out=tr[:, :], in_=p1[:, :]), reads=[p1], writes=[tr])
    for q in range(4):
        for kk in range(4):
            k = q * 4 + kk
            S.op("pe", lambda e, k=k, kk=kk: e.matmul(p2[:, kk * 128:(kk + 1) * 128], G.ident[0:16, k:k + 1].to_broadcast([16, 128]), tr[:, :],
                                                     start=True, stop=True), reads=[tr, G.cst], writes=[p2])
        S.op("act", lambda e, q=q: e.copy(out=o[:, q * 512:(q + 1) * 512], in_=p2[:, :]), reads=[p2], writes=[o])
    return o


def phase_wout(G, L, x_dram):
    S, nc, W = G.S, G.nc, L.W
    with ExitStack() as st:
        gbc = bcast_rows(G, st, (L.der.t[:, 32:48], L.der), "wo_gbc")
        wo = G.sb("wo_w", [128, 16, D], BF16, stack=st)
        wb = [Buf() for _ in range(4)]
        wv = W["w_out"].rearrange("(k p) n -> p k n", p=128)
        for q in range(4):
            S.dma("pool", wo[:, :, q * 512:(q + 1) * 512], wv[:, :, q * 512:(q + 1) * 512], writes=[wb[q]])
        mt = [G.sb("wo_m%d" % i, [128, 16, 128], BF16, stack=st) for i in range(2)]
        xt = [G.sb("wo_x%d" % i, [128, D], stack=st) for i in range(2)]
        xo = [G.sb("wo_o%d" % i, [128, D], stack=st) for i in range(2)]
        pp = [G.ps("wo_p%d" % i, [128, 512], stack=st) for i in range(2)]
        mview = G.mixT.rearrange("(k p) t -> p k t", p=128)
        for tt in range(NT):
            m_, x_, o_ = mt[tt % 2], xt[tt % 2], xo[tt % 2]
            S.dma("sp", m_[:, :, :], mview[:, :, tt * 128:(tt + 1) * 128], writes=[m_])
            S.dma("sp", x_[:, :], x_dram[tt * 128:(tt + 1) * 128, :], writes=[x_])
            for q in range(4):
                p_ = pp[q % 2]
                for k in range(16):
                    S.op("pe", lambda e, p_=p_, m_=m_, k=k, q=q: e.matmul(p_[:, :], m_[:, k, :], wo[:, k, q * 512:(q + 1) * 512], start=(k == 0), stop=(k == 15)),
                         reads=[m_, wb[q]], writes=[p_])
                S.op("dve", lambda e, p_=p_, o_=o_, q=q: e.tensor_tensor(out=o_[:, q * 512:(q + 1) * 512], in0=p_[:, :], in1=gbc[:, q * 512:(q + 1) * 512], op=ALU.mult),
                     reads=[p_, gbc], writes=[o_])
            S.op("pool", lambda e, o_=o_, x_=x_: e.tensor_tensor(out=o_[:, :], in0=o_[:, :], in1=x_[:, :], op=ALU.add), reads=[o_, x_], writes=[o_])
            S.dma("sp", G.xa[tt * 128:(tt + 1) * 128, :], o_[:, :], reads=[o_])
        S.barrier()


def phase_ffn(G, L):
    S, nc, W = G.S, G.nc, L.W
    moe = (L.l % 2 == 1)
    import os
    ne = int(os.environ.get('MOE_NE', NEXP)) if moe else 1
    with ExitStack() as st:
        hT = G.sb("ff_hT", [128, 16, 512], BF16, stack=st)
        acc = G.sb("ff_acc", [128, 16, 512], stack=st)
        act = G.sb("ff_act", [128, FC, 512], BF16, stack=st)
        actb = [Buf() for _ in range(FC)]
        wg = [G.sb("ff_wg%d" % i, [128, 16, 128], BF16, stack=st) for i in range(2)]
        wu = [G.sb("ff_wu%d" % i, [128, 16, 128], BF16, stack=st) for i in range(2)]
        wd = [G.sb("ff_wd%d" % i, [128, FC, 128], BF16, stack=st) for i in range(2)]
        sg = [G.sb("ff_sg%d" % i, [128, 512], stack=st) for i in range(2)]
        gbc = G.sb("ff_gbc", [128, 512], stack=st)
        xt = [G.sb("ff_x%d" % i, [128, D], stack=st) for i in range(2)]
        pg_ = [G.ps("ff_pg%d" % i, [128, 512], stack=st) for i in range(2)]
        pu_ = [G.ps("ff_pu%d" % i, [128, 512], stack=st) for i in range(2)]
        pd_ = [G.ps("ff_pd%d" % i, [128, 512], stack=st) for i in range(2)]
        if moe:
            gts = G.sb("ff_gts", [NEXP, T], stack=st)
            S.dma("sp", gts[:, :], G.gT, writes=[gts])
        hview = G.hT.rearrange("(k p) t -> p k t", p=128)
        wi = 0
        di = 0
        for q in range(4):
            ts_ = slice(q * 512, (q + 1) * 512)
            S.dma("sp", hT[:, :, :], hview[:, :, ts_], writes=[hT])
            for e_ in range(ne):
                if moe:
                    S.op("pe", lambda e, e_=e_, ts_=ts_: e.matmul(pd_[0][:, :], G.ident[0:NEXP, e_:e_ + 1].to_broadcast([NEXP, 128]), gts[:, ts_], start=True, stop=True),
                         reads=[gts, G.cst], writes=[pd_[0]])
                    S.op("act", lambda e: e.copy(out=gbc[:, :], in_=pd_[0][:, :]), reads=[pd_[0]], writes=[gbc])
                gv = W["ffn_gate"][e_].rearrange("(k p) f -> p k f", p=128)
                uv = W["ffn_up"][e_].rearrange("(k p) f -> p k f", p=128)
                dv = W["ffn_down"][e_].rearrange("(c p) d -> p c d", p=128)
                for fc in range(FC):
                    g_, u_ = wg[wi % 2], wu[wi % 2]
                    pg, pu, s_ = pg_[wi % 2], pu_[wi % 2], sg[wi % 2]
                    wi += 1
                    S.dma("pool", g_[:, :, :], gv[:, :, fc * 128:(fc + 1) * 128], writes=[g_])
                    S.dma("pool", u_[:, :, :], uv[:, :, fc * 128:(fc + 1) * 128], writes=[u_])
                    for k in range(16):
                        S.op("pe", lambda e, pg=pg, g_=g_, k=k: e.matmul(pg[:, :], g_[:, k, :], hT[:, k, :], start=(k == 0), stop=(k == 15)), reads=[g_, hT], writes=[pg])
                    for k in range(16):
                        S.op("pe", lambda e, pu=pu, u_=u_, k=k: e.matmul(pu[:, :], u_[:, k, :], hT[:, k, :], start=(k == 0), stop=(k == 15)), reads=[u_, hT], writes=[pu])
                    S.op("act", lambda e, pg=pg, s_=s_: e.activation(out=s_[:, :], in_=pg[:, :], func=AF.Silu), reads=[pg], writes=[s_])
                    if moe:
                        S.op("dve", lambda e, s_=s_: e.tensor_tensor(out=s_[:, :], in0=s_[:, :], in1=gbc[:, :], op=ALU.mult), reads=[s_, gbc], writes=[s_])
                    S.op("dve", lambda e, s_=s_, pu=pu, fc=fc: e.tensor_tensor(out=act[:, fc, :], in0=s_[:, :], in1=pu[:, :], op=ALU.mult), reads=[s_, pu], writes=[actb[fc]])
                for dc in range(16):
                    d_ = wd[di % 2]
                    pd = pd_[di % 2]
                    di += 1
                    S.dma("pool", d_[:, :, :], dv[:, :, dc * 128:(dc + 1) * 128], writes=[d_])
                    for fc in range(FC):
                        S.op("pe", lambda e, pd=pd, d_=d_, fc=fc: e.matmul(pd[:, :], d_[:, fc, :], act[:, fc, :], start=(fc == 0), stop=(fc == FC - 1)),
                             reads=[d_, actb[fc]], writes=[pd])
                    if e_ == 0:
                        S.op("act", lambda e, pd=pd, dc=dc: e.copy(out=acc[:, dc, :], in_=pd[:, :]), reads=[pd], writes=[acc])
                    else:
                        S.op("dve", lambda e, pd=pd, dc=dc: e.tensor_tensor(out=acc[:, dc, :], in0=acc[:, dc, :], in1=pd[:, :], op=ALU.add), reads=[pd, acc], writes=[acc])
            for dc in range(16):
                S.op("act", lambda e, dc=dc: e.activation(out=acc[:, dc, :], in_=acc[:, dc, :], func=AF.Copy, scale=L.der[:, 80 + dc:81 + dc]), reads=[acc, L.der], writes=[acc])
            for s4 in range(4):
                tt = q * 4 + s4
                x_ = xt[s4 % 2]
                S.dma("sp", x_[:, :], G.xa[tt * 128:(tt + 1) * 128, :], writes=[x_])
                for d4 in range(4):
                    p_ = pu_[d4 % 2]
                    for dd in range(4):
                        dc = d4 * 4 + dd
                        S.op("pe", lambda e, p_=p_, dd=dd, dc=dc, s4=s4: e.transpose(out=p_[:, dd * 128:(dd + 1) * 128], in_=acc[:, dc, s4 * 128:(s4 + 1) * 128], identity=G.ident),
                             reads=[acc, G.cst], writes=[p_])
                    S.op("dve", lambda e, p_=p_, x_=x_, d4=d4: e.tensor_tensor(out=x_[:, d4 * 512:(d4 + 1) * 512], in0=x_[:, d4 * 512:(d4 + 1) * 512], in1=p_[:, :], op=ALU.add),
                         reads=[p_, x_], writes=[x_])
                S.dma("sp", G.xb[tt * 128:(tt + 1) * 128, :], x_[:, :], reads=[x_])
        S.barrier()


def phase_final(G, x_dram):
    S, nc = G.S, G.nc
    with ExitStack() as st:
        xt = [G.sb("fn_x%d" % i, [128, D], stack=st) for i in range(2)]
        sq = G.sb("fn_sq", [128, D], stack=st)
        ss = [G.sb("fn_ss%d" % i, [128, 2], stack=st) for i in range(2)]
        gn = G.sb("fn_g", [128, D], stack=st)
        S.dma("sp", gn[:, :], G.rowp[:, RO_NOUT:RO_NOUT + D], writes=[gn])
        for tt in range(NT):
            x, s_ = xt[tt % 2], ss[tt % 2]
            S.dma("sp", x[:, :], x_dram[tt * 128:(tt + 1) * 128, :], writes=[x])
            S.op("act", lambda e, x=x: e.activation(out=sq[:, :], in_=x[:, :], func=AF.Square), reads=[x], writes=[sq])
            S.op("dve", lambda e, s_=s_: e.tensor_reduce(out=s_[:, 0:1], in_=sq[:, :], axis=AX.X, op=ALU.add), reads=[sq], writes=[s_])
            S.op("act", lambda e, s_=s_: e.activation(out=s_[:, 1:2], in_=s_[:, 0:1], func=AF.Sqrt, scale=1.0 / D, bias=G.cst.t[:, CO_EPS6:CO_EPS6 + 1]),
                 reads=[s_, G.cst], writes=[s_])
            S.op("dve", lambda e, s_=s_: e.reciprocal(out=s_[:, 0:1], in_=s_[:, 1:2]), reads=[s_], writes=[s_])
            S.op("dve", lambda e, x=x, s_=s_: e.scalar_tensor_tensor(out=x[:, :], in0=x[:, :], scalar=s_[:, 0:1], in1=gn[:, :], op0=ALU.mult, op1=ALU.mult),
                 reads=[x, s_, gn], writes=[x])
            S.dma("sp", G.out[tt * 128:(tt + 1) * 128, :], x[:, :], reads=[x])
        S.barrier()


def make_in_map(inp, b, n_layers=2):
    m = {"x": np.ascontiguousarray(inp["x"][b]), "consts": make_consts(), "rowp": make_rowp(inp)}
    for l in range(n_layers):
        p = "l%d_" % l
        m[p + "vecs"] = make_vecs(inp, l, b)
        for k in ("mod_w", "w_in", "w2", "a2", "g2", "w_out"):
            m[p + k] = np.ascontiguousarray(inp[p + k])
        if l > 0:
            m[p + "v2"] = np.ascontiguousarray(inp[p + "v2"])
        if l % 2 == 0:
            for k in ("ffn_gate", "ffn_up", "ffn_down"):
                m[p + k] = np.ascontiguousarray(inp[p + k])[None]
        else:
            import os
            nin = int(os.environ.get("DEV_NEXP", NEXP))
            m[p + "router"] = np.ascontiguousarray(inp[p + "router"].reshape(16, 128, NEXP).transpose(1, 0, 2).reshape(128, 16 * NEXP))
            for k in ("exp_gate", "exp_up", "exp_down"):
                m[p + k] = np.ascontiguousarray(inp[p + k][:nin])
    return m


def kernel(**inputs):
    inp = {k: np.asarray(v) for k, v in inputs.items()}
    nc = build_program()
    maps = [make_in_map(inp, b) for b in range(4)]
    in_maps = [maps[c % 4] for c in range(N_CORES)]
    res = run_bass_kernel_spmd(nc, in_maps, core_ids=list(range(N_CORES)))
    out = np.stack([np.asarray(res.results[b]["out"]) for b in range(4)], axis=0)
    return out.astype(np.float32)
```
